# Optimizing a Trainium2 kernel written in Bass

```python
import math
import jax, jax.numpy as jnp
from jax import lax
import numpy as np

D_MODEL = 1024
BATCH = 8
SEQ = 4096
DEPTH = 1

CHUNK = 64
EPS = 1e-6
H_A = 8
DK_A = 128
DV_A = 128
CONV_K = 4
H_B = 8
D_HEAD_B = 128
H_IDX = 8
D_IDX = 64
TOPK_MAX = 256
Q_BLOCK = 128
N_BUCKETS = 32
MAX_DISTANCE = 128
D_FF = 4 * D_MODEL
N_BRANCH = 2

SPLIT_SIZES = (H_A * DK_A, H_A * DK_A, H_A * DV_A, H_A * DV_A, H_A, H_A,
               H_B * D_HEAD_B, H_B * D_HEAD_B, H_B * D_HEAD_B, H_IDX * D_IDX, D_IDX, H_IDX)
D_IN = sum(SPLIT_SIZES)

kernel_name = 'hybrid_gdn_dsa_streaming_block'


def _rmsnorm(x, w):
    xf = x.astype(jnp.float32)
    y = xf * lax.rsqrt(jnp.mean(xf * xf, axis=-1, keepdims=True) + EPS)
    return (y * w.astype(jnp.float32)).astype(x.dtype)


def _l2norm(x):
    xf = x.astype(jnp.float32)
    return xf * lax.rsqrt(jnp.sum(xf * xf, axis=-1, keepdims=True) + EPS)


def _causal_depthwise_conv(x, w):
    return lax.conv_general_dilated(
        x, w[:, None, :].astype(x.dtype), window_strides=(1,), padding=[(CONV_K - 1, 0)],
        dimension_numbers=('NWC', 'WIO', 'NWC'), feature_group_count=x.shape[-1])


def _t5_bucket(rel):
    half = N_BUCKETS // 2
    max_exact = half // 2
    base = jnp.where(rel > 0, half, 0)
    n = jnp.abs(rel)
    n_f = jnp.maximum(n, 1).astype(jnp.float32)
    large = max_exact + (jnp.log(n_f / max_exact) / math.log(MAX_DISTANCE / max_exact)
                         * (half - max_exact)).astype(jnp.int32)
    large = jnp.minimum(large, half - 1)
    return base + jnp.where(n < max_exact, n, large)


def _gated_delta_rule(q, k, v, g, beta):
    b_, t_, h_, dk = q.shape
    dv = v.shape[-1]
    n = t_ // CHUNK

    def chunks(a):
        return a.astype(jnp.float32).reshape(b_, n, CHUNK, h_, -1).transpose(1, 0, 3, 2, 4)

    q, k, v = chunks(q), chunks(k), chunks(v)
    g = jnp.cumsum(chunks(g[..., None])[..., 0], axis=-1)
    beta = chunks(beta[..., None])[..., 0]
    causal_incl = jnp.tril(jnp.ones((CHUNK, CHUNK), dtype=bool))
    strict = jnp.tril(jnp.ones((CHUNK, CHUNK), dtype=bool), k=-1)
    diff = g[..., :, None] - g[..., None, :]
    decay = jnp.where(causal_incl, jnp.exp(jnp.where(causal_incl, diff, 0.0)), 0.0)
    k_beta = k * beta[..., None]
    lower = jnp.where(strict, jnp.einsum('nbhcd,nbhsd->nbhcs', k_beta, k) * decay, 0.0)
    eye = jnp.eye(CHUNK, dtype=jnp.float32)
    rhs = jnp.concatenate([v * beta[..., None], k_beta * jnp.exp(g)[..., None]], axis=-1)
    sol = lax.linalg.triangular_solve(lower + eye, rhs, left_side=True, lower=True, unit_diagonal=True)
    u, w = sol[..., :dv], sol[..., dv:]
    qk = jnp.where(causal_incl, jnp.einsum('nbhcd,nbhsd->nbhcs', q, k) * decay, 0.0)
    q_dec = q * jnp.exp(g)[..., None]
    g_last = g[..., -1]
    k_tail = k * jnp.exp(g_last[..., None] - g)[..., None]

    def step(S, xs):
        u_i, w_i, qk_i, q_dec_i, k_tail_i, g_last_i = xs
        v_new = u_i - jnp.einsum('bhcd,bhde->bhce', w_i, S)
        o_i = jnp.einsum('bhcd,bhde->bhce', q_dec_i, S) + jnp.einsum('bhcs,bhse->bhce', qk_i, v_new)
        S = S * jnp.exp(g_last_i)[..., None, None] + jnp.einsum('bhcd,bhce->bhde', k_tail_i, v_new)
        return S, o_i

    S0 = jnp.zeros((b_, h_, dk, dv), jnp.float32)
    _, o = lax.scan(step, S0, (u, w, qk, q_dec, k_tail, g_last))
    return o.transpose(1, 0, 3, 2, 4).reshape(b_, t_, h_, dv)


def _dsa_attention(q, k, v, iq, ik, iw, rel_table):
    t_ = q.shape[1]
    topk = min(TOPK_MAX, t_ // 4)
    n_blocks = t_ // Q_BLOCK
    key_chunk = jnp.arange(t_, dtype=jnp.int32) // CHUNK
    scale = D_HEAD_B ** -0.5

    def per_sequence(args):
        q_s, k_s, v_s, iq_s, ik_s, iw_s = args
        ik_f = ik_s.astype(jnp.float32)

        def per_block(blk):
            start = blk * Q_BLOCK
            qb = lax.dynamic_slice_in_dim(q_s, start, Q_BLOCK, axis=0)
            iqb = lax.dynamic_slice_in_dim(iq_s, start, Q_BLOCK, axis=0).astype(jnp.float32)
            iwb = lax.dynamic_slice_in_dim(iw_s, start, Q_BLOCK, axis=0).astype(jnp.float32)
            q_pos = start + jnp.arange(Q_BLOCK, dtype=jnp.int32)
            score = jnp.einsum('qh,qhs->qs', iwb, jax.nn.relu(jnp.einsum('qhd,sd->qhs', iqb, ik_f)))
            visible = key_chunk[None, :] <= (q_pos // CHUNK)[:, None]
            score = jnp.where(visible, score, -jnp.inf)
            top_val, top_idx = lax.top_k(score, topk)
            valid = jnp.isfinite(top_val)
            kg = k_s[top_idx]
            vg = v_s[top_idx]
            logits = jnp.einsum('qhd,qkhd->qhk', qb, kg).astype(jnp.float32) * scale
            bias = rel_table[_t5_bucket(top_idx - q_pos[:, None])].astype(jnp.float32)
            logits = jnp.where(valid[:, None, :], logits + bias.transpose(0, 2, 1), -jnp.inf)
            p = jax.nn.softmax(logits, axis=-1)
            return jnp.einsum('qhk,qkhd->qhd', p.astype(vg.dtype), vg)

        out = lax.map(per_block, jnp.arange(n_blocks, dtype=jnp.int32))
        return out.reshape(t_, q_s.shape[1], q_s.shape[2])

    return lax.map(per_sequence, (q, k, v, iq, ik, iw))


def setup_inputs(seed: int = 0) -> dict:
    key = jax.random.key(seed)
    ks = jax.random.split(key, 17)
    f32 = jnp.float32
    nrm = lambda k_, shape, s: jax.random.normal(k_, shape, f32) * s
    return {
        'x': jax.random.normal(ks[0], (BATCH, SEQ, D_MODEL), f32),
        'norm1_w': 1.0 + nrm(ks[1], (DEPTH, D_MODEL), 0.05),
        'w_in': nrm(ks[2], (DEPTH, D_MODEL, D_IN), D_MODEL ** -0.5),
        'conv_a_w': nrm(ks[3], (DEPTH, CONV_K, 2 * H_A * DK_A + H_A * DV_A), CONV_K ** -0.5),
        'a_log': jnp.log(jax.random.uniform(ks[4], (DEPTH, H_A), f32, 1.0, 16.0)),
        'dt_bias': jnp.log(jnp.expm1(jax.random.uniform(ks[5], (DEPTH, H_A), f32, 1e-3, 0.1))),
        'norm_a_w': 1.0 + nrm(ks[6], (DEPTH, DV_A), 0.05),
        'rel_bias_table': nrm(ks[7], (N_BUCKETS, H_B), 0.5),
        'w_gate': nrm(ks[8], (DEPTH, D_MODEL, N_BRANCH * D_MODEL), D_MODEL ** -0.5),
        'b_gate': nrm(ks[9], (DEPTH, N_BRANCH * D_MODEL), 0.01),
        'w_proj_a': nrm(ks[10], (DEPTH, H_A * DV_A, D_MODEL), (H_A * DV_A) ** -0.5),
        'w_proj_b': nrm(ks[11], (DEPTH, H_B * D_HEAD_B, D_MODEL), (H_B * D_HEAD_B) ** -0.5),
        'w_out': nrm(ks[12], (DEPTH, D_MODEL, D_MODEL), D_MODEL ** -0.5),
        'norm2_w': 1.0 + nrm(ks[13], (DEPTH, D_MODEL), 0.05),
        'w_ff1': nrm(ks[14], (DEPTH, D_MODEL, D_FF), D_MODEL ** -0.5),
        'w_ff2': nrm(ks[15], (DEPTH, D_FF, D_MODEL), D_FF ** -0.5),
        'norm_final_w': 1.0 + nrm(ks[16], (D_MODEL,), 0.05),
    }


def reference(x, norm1_w, w_in, conv_a_w, a_log, dt_bias, norm_a_w, rel_bias_table, w_gate, b_gate,
              w_proj_a, w_proj_b, w_out, norm2_w, w_ff1, w_ff2, norm_final_w):
    b_, t_, _ = x.shape
    f32 = jnp.float32
    split_points = np.cumsum(SPLIT_SIZES)[:-1].tolist()
    for layer in range(DEPTH):
        h = _rmsnorm(x, norm1_w[layer])
        qa, ka, va, za, ba, aa, qb, kb, vb, iq, ik, iw = jnp.split(h @ w_in[layer], split_points, axis=-1)

        qkv_a = jax.nn.silu(_causal_depthwise_conv(jnp.concatenate([qa, ka, va], axis=-1), conv_a_w[layer]))
        qa, ka, va = jnp.split(qkv_a, [H_A * DK_A, 2 * H_A * DK_A], axis=-1)
        qa = _l2norm(qa.reshape(b_, t_, H_A, DK_A)) * (DK_A ** -0.5)
        ka = _l2norm(ka.reshape(b_, t_, H_A, DK_A))
        beta = jax.nn.sigmoid(ba.astype(f32))
        g = -jnp.exp(a_log[layer].astype(f32)) * jax.nn.softplus(aa.astype(f32) + dt_bias[layer].astype(f32))
        oa = _gated_delta_rule(qa, ka, va.reshape(b_, t_, H_A, DV_A), g, beta).astype(x.dtype)
        oa = (_rmsnorm(oa, norm_a_w[layer]) * jax.nn.silu(za.reshape(b_, t_, H_A, DV_A))).reshape(b_, t_, H_A * DV_A)

        ob = _dsa_attention(qb.reshape(b_, t_, H_B, D_HEAD_B), kb.reshape(b_, t_, H_B, D_HEAD_B),
                            vb.reshape(b_, t_, H_B, D_HEAD_B), iq.reshape(b_, t_, H_IDX, D_IDX), ik, iw,
                            rel_bias_table).reshape(b_, t_, H_B * D_HEAD_B)

        gates = jax.nn.sigmoid(h @ w_gate[layer] + b_gate[layer]).reshape(b_, t_, N_BRANCH, D_MODEL)
        merged = gates[:, :, 0, :] * (oa @ w_proj_a[layer]) + gates[:, :, 1, :] * (ob @ w_proj_b[layer])
        x = x + merged @ w_out[layer]

        h2 = _rmsnorm(x, norm2_w[layer])
        x = x + jnp.square(jax.nn.relu(h2 @ w_ff1[layer])) @ w_ff2[layer]
    return _rmsnorm(x, norm_final_w)
```

```python
from contextlib import ExitStack
import math
import numpy as np
import ml_dtypes
import concourse.bass as bass
import concourse.mybir as mybir
from concourse.bass_utils import run_bass_kernel_spmd

F32 = mybir.dt.float32
BF16 = mybir.dt.bfloat16
AF = mybir.ActivationFunctionType
ALU = mybir.AluOpType
AX = mybir.AxisListType

T = 4096
D = 1024
NT = T // 128
NB = T // 512
DIN = 7768
EPS = 1e-6
C_QA, C_KA, C_VA, C_ZA, C_BA, C_AA, C_QB, C_KB, C_VB, C_IQ, C_IK, C_IW = (
    0, 1024, 2048, 3072, 4096, 4104, 4112, 5136, 6160, 7184, 7696, 7760)


class Ctx:
    def __init__(self, nc):
        self.nc = nc
        self.eng = {'pe': nc.tensor, 'act': nc.scalar, 'dve': nc.vector, 'pool': nc.gpsimd, 'sp': nc.sync}
        self.esem = {k: nc.alloc_semaphore(name='s_' + k) for k in ('pe', 'act', 'dve', 'pool')}
        self.ecnt = {k: 0 for k in self.esem}
        self.seen = {k: {} for k in self.eng}
        self.W = {}
        self.R = {}
        self.dsem = {}
        self.BK = {}
        self.bank_of = {}
        self.nwait = 0
        self.nins = 0

    def _deps(self, reads, writes):
        deps = {}

        def add(d):
            for name, (sem, val) in d.items():
                if name not in deps or deps[name][1] < val:
                    deps[name] = (sem, val)
        for k in reads:
            add(self.W.get(k, {}))
        for k in writes:
            add(self.W.get(k, {}))
            add(self.R.get(k, {}))
        return deps

    def _wait(self, e, deps):
        for name, (sem, val) in deps.items():
            if e == 'pe' and name == 'pe':
                continue
            if self.seen[e].get(name, 0) >= val:
                continue
            self.eng[e].wait_ge(sem, val)
            self.seen[e][name] = val
            self.nwait += 1

    def _commit(self, name, sem, val, reads, writes):
        for k in writes:
            self.W[k] = {name: (sem, val)}
            self.R[k] = {}
        for k in reads:
            self.R.setdefault(k, {})[name] = (sem, val)

    def op(self, e, fn, reads=(), writes=()):
        deps = self._deps(reads, writes)
        banks = {self.bank_of[k] for k in list(reads) + list(writes) if k in self.bank_of}
        for bk in banks:
            for name, (sem, val) in self.BK.get(bk, {}).items():
                if name != e and (name not in deps or deps[name][1] < val):
                    deps[name] = (sem, val)
        self._wait(e, deps)
        ins = fn(self.eng[e])
        self.ecnt[e] += 1
        ins.then_inc(self.esem[e], 1)
        self.nins += 1
        self._commit(e, self.esem[e], self.ecnt[e], reads, writes)
        for bk in banks:
            self.BK.setdefault(bk, {})[e] = (self.esem[e], self.ecnt[e])

    def dma(self, q, out, in_, reads, writes, slot, **kw):
        slot = f'{q}_{slot}'
        self._wait(q, self._deps(reads, writes))
        if slot not in self.dsem:
            self.dsem[slot] = [self.nc.alloc_semaphore(name='d_' + slot), 0]
        ds = self.dsem[slot]
        if ds[1] > 0 and self.seen[q].get('d_' + slot, 0) < ds[1]:
            self.eng[q].wait_ge(ds[0], ds[1])
            self.seen[q]['d_' + slot] = ds[1]
            self.nwait += 1
        ins = self.eng[q].dma_start(out=out, in_=in_, **kw)
        ds[1] += 16
        ins.then_inc(ds[0], 16)
        self.nins += 1
        self._commit('d_' + slot, ds[0], ds[1], reads, writes)

    def barrier(self, engines=('pe', 'act', 'dve', 'pool', 'sp')):
        allsem = {k: (self.esem[k], self.ecnt[k]) for k in self.esem if self.ecnt[k] > 0}
        for s, (sem, v) in self.dsem.items():
            if v > 0:
                allsem['d_' + s] = (sem, v)
        for e in engines:
            for name, (sem, val) in allsem.items():
                if e == name:
                    continue
                if self.seen[e].get(name, 0) >= val:
                    continue
                self.eng[e].wait_ge(sem, val)
                self.seen[e][name] = val
        self.W.clear()
        self.R.clear()
        self.BK.clear()


def _consts():
    c = {}
    c['ident'] = np.eye(128, dtype=np.float32)
    sel = np.zeros((48, 32, 128), np.float32)
    for q, base in enumerate((16, 40, 8, 0)):
        for h in range(8):
            sel[base + h, q * 8 + h, :] = 1.0
    c['sel'] = sel
    i = np.arange(128)
    same = (i[:, None] // 64) == (i[None, :] // 64)
    NEG = -30000.0
    negS = np.where(same & (i[:, None] < i[None, :]), 0.0, NEG)
    negI = np.where(same & (i[:, None] <= i[None, :]), 0.0, NEG)
    negST = np.where(same & (i[None, :] < i[:, None]), 0.0, NEG)
    rel = (511 - np.arange(1152)).astype(np.int32)
    half, max_exact = 16, 8
    base = np.where(rel > 0, half, 0)
    nn = np.abs(rel)
    n_f = np.maximum(nn, 1).astype(np.float32)
    large = max_exact + (np.log(n_f / np.float32(max_exact)) / np.float32(math.log(128 / max_exact))
                         * np.float32(half - max_exact)).astype(np.int32)
    large = np.minimum(large, half - 1)
    bucket = base + np.where(nn < max_exact, nn, large)
    ohm = np.zeros((32, 1152), np.float32)
    ohm[bucket, np.arange(1152)] = 1.0
    c['oh'] = ohm
    c['J'] = np.ascontiguousarray(np.eye(128, dtype=np.float32)[::-1])
    tl = np.arange(128)
    c['visneg'] = np.where((tl[:, None] < 64) & (tl[None, :] >= 64), -30000.0, 0.0).astype(np.float32)
    c['pw'] = np.ascontiguousarray(np.broadcast_to((2.0 ** -np.arange(1, 17)).astype(np.float32)[None, :], (128, 16)))
    c['negm'] = np.ascontiguousarray(np.stack([negS, negI, negST], axis=1).astype(np.float32))
    return c


def build_program(debug=False):
    nc = bass.Bass("TRN2", target_bir_lowering=False)
    ikind = "ExternalOutput" if debug else "Internal"

    def din(name, shape, dt=F32):
        return nc.dram_tensor(name, list(shape), dt, kind="ExternalInput").ap()

    def dscr(name, shape, dt=BF16):
        return nc.dram_tensor(name, list(shape), dt, kind=ikind).ap()

    x_d = din("x", [T, D])
    n1w_d = din("norm1_w", [1, D])
    w_in_d = din("w_in", [D, DIN])
    convw_d = din("conv_wt", [3072, 4])
    alog_d = din("a_log", [8, 1])
    dtb_d = din("dt_bias", [8, 1])
    naw_d = din("norm_a_w", [1, 128])
    rel_d = din("rel_bias_table", [32, 8])
    wg_d = din("w_gate", [D, 2048])
    bg_d = din("b_gate_t", [128, 16])
    wpa_d = din("w_proj_a", [D, D])
    wpb_d = din("w_proj_b", [D, D])
    wo_d = din("w_out", [D, D])
    n2w_d = din("norm2_w", [1, D])
    wf1_d = din("w_ff1", [D, 4096])
    wf2_d = din("w_ff2", [4096, D])
    nfw_d = din("norm_final_w", [1, D])
    ident_d = din("c_ident", [128, 128])
    sel_d = din("c_sel", [48, 32, 128])
    negm_d = din("c_negm", [128, 3, 128])
    oh_d = din("c_oh", [32, 1152])
    J_d = din("c_J", [128, 128])
    visneg_d = din("c_visneg", [128, 128])
    pw_d = din("c_pw", [128, 16])
    y_d = nc.dram_tensor("y", [T, D], F32, kind="ExternalOutput").ap()

    qaT_s = dscr("qaT_s", [1024, T])
    kaT_s = dscr("kaT_s", [1024, T])
    vaT_s = dscr("vaT_s", [1024, T])
    za_s = dscr("za_s", [T, 1024])
    qbT_s = dscr("qbT_s", [1024, T])
    kbT_s = dscr("kbT_s", [1024, T])
    vb_s = dscr("vb_s", [T, 1024])
    iqT_s = dscr("iqT_s", [512, T])
    ikT_s = dscr("ikT_s", [128, T])
    gT_s = dscr("gT_s", [2048, T])
    gate_s = dscr("gate_s", [2, 8, T], F32)
    oa_s = dscr("oa_s", [T, 1024])
    ob_s = dscr("ob_s", [T, 1024])
    fr_s = dscr("fr_s", [8, 1152], F32)
    x1_s = dscr("x1_s", [T, D], F32)
    h2T_s = dscr("h2T_s", [128, 8, T])
    dbg = {}
    if debug:
        dbg['hT'] = nc.dram_tensor("dbg_hT", [128, 8, T], BF16, kind="ExternalOutput").ap()
        dbg['iw'] = nc.dram_tensor("dbg_iw", [128, NT, 8], F32, kind="ExternalOutput").ap()

    c = Ctx(nc)
    top = ExitStack()

    def sb(es, name, shape, dt):
        return es.enter_context(nc.sbuf_tensor(name, list(shape), dt))

    def ps(es, name, shape, dt):
        return es.enter_context(nc.psum_tensor(name, list(shape), dt))

    with top:
        ident_f = sb(top, "ident_f", [128, 128], F32)
        ident_b = sb(top, "ident_b", [128, 128], BF16)
        ones_b = sb(top, "ones_b", [128, 128], BF16)
        eps_t = sb(top, "eps_t", [128, 1], F32)
        mhalf_t = sb(top, "mhalf_t", [128, 1], F32)
        iw_all = sb(top, "iw_all", [128, NT, 8], F32)
        gdn_top = ExitStack()
        SRall = sb(gdn_top, "SRall", [48, T], F32)
        tokS = sb(gdn_top, "tokS", [128, NT, 48], F32)
        eglast = sb(gdn_top, "eglast", [128, 8, 64], F32)
        c.dma('sp', ident_f[:], ident_d, [], ['ident_f'], 'c0')
        c.dma('pool', ident_b[:], ident_d, [], ['ident_b'], 'c1')
        c.op('dve', lambda e: e.memset(ones_b[:], 1.0), [], ['ones_b'])
        c.op('dve', lambda e: e.memset(eps_t[:], EPS), [], ['eps_t'])
        c.op('dve', lambda e: e.memset(mhalf_t[:], -0.5), [], ['mhalf_t'])

        with ExitStack() as p12:
            hT = sb(p12, "hT", [128, 8, T], BF16)
            with ExitStack() as p1:
                n1w_bc = sb(p1, "n1w_bc", [128, D], F32)
                c.dma('sp', n1w_bc[:], n1w_d.partition_broadcast(128), [], ['n1w_bc'], 'c2')
                xt = [sb(p1, f"xt{i}", [128, D], F32) for i in range(3)]
                hb = [sb(p1, f"hb{i}", [128, D], BF16) for i in range(2)]
                junk = sb(p1, "junk1", [128, D], BF16)
                ss = sb(p1, "ss", [128, NT], F32)
                rs = sb(p1, "rs", [128, NT], F32)
                rstd = sb(p1, "rstd", [128, NT], F32)
                pst = [ps(p1, f"pst{i}", [128, 8, 128], BF16) for i in range(2)]

                def stA(i):
                    s = i % 3
                    c.dma('sp', xt[s][:], x_d[i * 128:(i + 1) * 128, :], [], [f'xt{s}'], f'xt{s}')
                    c.op('act', lambda e: e.activation(out=junk[:], in_=xt[s][:], func=AF.Square,
                                                       accum_out=ss[:, i:i + 1]), [f'xt{s}'], [f'ss{i}', 'junk1'])

                def stB(i):
                    s = i % 3
                    c.op('pool', lambda e: e.tensor_scalar(out=rs[:, i:i + 1], in0=ss[:, i:i + 1],
                                                           scalar1=1.0 / D, scalar2=EPS, op0=ALU.mult, op1=ALU.add),
                         [f'ss{i}'], [f'rs{i}'])
                    c.op('pool', lambda e: e.tensor_tensor(out=rstd[:, i:i + 1], in0=rs[:, i:i + 1],
                                                           in1=mhalf_t[:], op=ALU.pow),
                         [f'rs{i}', 'mhalf_t'], [f'rstd{i}'])
                    c.op('dve', lambda e: e.scalar_tensor_tensor(out=hb[i % 2][:], in0=xt[s][:],
                                                                 scalar=rstd[:, i:i + 1], in1=n1w_bc[:],
                                                                 op0=ALU.mult, op1=ALU.mult),
                         [f'xt{s}', f'rstd{i}', 'n1w_bc'], [f'hb{i % 2}'])

                def stC(i):
                    b = i % 2
                    for k in range(8):
                        c.op('pe', lambda e: e.transpose(out=pst[b][:, k, :], in_=hb[b][:, k * 128:(k + 1) * 128],
                                                         identity=ident_b[:]),
                             [f'hb{b}', 'ident_b'], [f'pst{b}'])
                    c.op('act', lambda e: e.copy(out=hT[:, :, i * 128:(i + 1) * 128], in_=pst[b][:]),
                         [f'pst{b}'], [f'hT{i // 4}'])

                for i in range(NT + 2):
                    if i < NT:
                        stA(i)
                    if 0 <= i - 1 < NT:
                        stB(i - 1)
                    if 0 <= i - 2 < NT:
                        stC(i - 2)
                c.barrier()
            if debug:
                c.dma('sp', dbg['hT'], hT[:], [f'hT{b}' for b in range(NB)], [], 'dbg0')

            with ExitStack() as p2:
                wch = [sb(p2, f"wch{i}", [128, 8, 512], BF16) for i in range(2)]
                stg = [sb(p2, f"stg{i}", [128, T], BF16) for i in range(2)]
                tstg = [sb(p2, f"tstg{i}", [128, 512], BF16) for i in range(3)]
                pres = [sb(p2, f"pre{i}", [128, T + 4], F32) for i in range(2)]
                accs = [sb(p2, f"acc{i}", [128, T], F32) for i in range(2)]
                pre, acc = pres[0], accs[0]
                sq = sb(p2, "sq", [128, T], BF16)
                cw = sb(p2, "cw", [128, 24, 4], F32)
                bg = sb(p2, "bg", [128, 16], F32)
                psa = [ps(p2, f"psa{i}", [128, 512], F32) for i in range(3)]
                psn = [ps(p2, f"psn{i}", [128, 512], F32) for i in range(2)]
                c.dma('sp', cw[:], convw_d.rearrange("(c p) j -> p c j", p=128), [], ['cw'], 'c3')
                c.dma('sp', bg[:], bg_d, [], ['bg'], 'c4')
                c.op('dve', lambda e: e.memset(pres[0][:, 0:3], 0.0), [], ['pre0'])
                c.op('dve', lambda e: e.memset(pres[1][:, 0:3], 0.0), [], ['pre1'])
                state = {'w': 0, 'ps': 0, 'stg': 0, 'ts': 0, 'ev': 0}

                def load_w(src_d, col0, ncols, dst_col=0, slot=None):
                    s = state['w'] % 2 if slot is None else slot
                    if slot is None:
                        state['w'] += 1
                    c.dma('pool', wch[s][:, :, dst_col:dst_col + ncols],
                          src_d.rearrange("(k p) c -> p k c", p=128)[:, :, col0:col0 + ncols],
                          [], [f'wch{s}'], f'wch{s}')
                    return s

                def fm_block(ws, wc0, m, blk):
                    p = state['ps'] % 3
                    state['ps'] += 1
                    for k in range(8):
                        c.op('pe', lambda e: e.matmul(psa[p][0:m, :], lhsT=wch[ws][:, k, wc0:wc0 + m],
                                                      rhs=hT[:, k, blk * 512:(blk + 1) * 512],
                                                      start=(k == 0), stop=(k == 7)),
                             [f'wch{ws}', f'hT{blk}'], [f'psa{p}'])
                    return p

                def evac_copy(out_ap, in_ap, rkeys, wkeys):
                    if state['ev'] % 2 == 0:
                        c.op('act', lambda e: e.copy(out=out_ap, in_=in_ap), rkeys, wkeys)
                    else:
                        c.op('dve', lambda e: e.tensor_copy(out=out_ap, in_=in_ap), rkeys, wkeys)
                    state['ev'] += 1

                def fm_plain(src_d, col0, nchunks, dst_s, row0=0):
                    for g0 in range(0, nchunks, 4):
                        gn = min(4, nchunks - g0)
                        ws = load_w(src_d, col0 + g0 * 128, gn * 128)
                        for cc in range(gn):
                            s = state['stg'] % 2
                            state['stg'] += 1
                            for blk in range(NB):
                                p = fm_block(ws, cc * 128, 128, blk)
                                evac_copy(stg[s][:, blk * 512:(blk + 1) * 512], psa[p][:],
                                          [f'psa{p}'], [f'stg{s}'])
                            r = row0 + (g0 + cc) * 128
                            c.dma('sp', dst_s[r:r + 128, :], stg[s][:], [f'stg{s}'], [dst_s.tensor.name], f'stg{s}')

                fm_plain(w_in_d, C_QB, 8, qbT_s)
                fm_plain(w_in_d, C_KB, 8, kbT_s)
                fm_plain(w_in_d, C_IQ, 4, iqT_s)
                ws = state['w'] % 2
                state['w'] += 1
                load_w(w_in_d, C_IK, 64, dst_col=0, slot=ws)
                load_w(w_in_d, C_IK, 64, dst_col=64, slot=ws)
                s = state['stg'] % 2
                state['stg'] += 1
                for blk in range(NB):
                    p = fm_block(ws, 0, 128, blk)
                    evac_copy(stg[s][:, blk * 512:(blk + 1) * 512], psa[p][:], [f'psa{p}'], [f'stg{s}'])
                c.dma('sp', ikT_s[:, :], stg[s][:], [f'stg{s}'], ['ikT_s'], f'stg{s}')
                for g0 in range(0, 16, 4):
                    ws = load_w(wg_d, g0 * 128, 512)
                    for cc in range(4):
                        s = state['stg'] % 2
                        state['stg'] += 1
                        ch = g0 + cc
                        for blk in range(NB):
                            p = fm_block(ws, cc * 128, 128, blk)
                            c.op('act', lambda e: e.activation(out=stg[s][:, blk * 512:(blk + 1) * 512], in_=psa[p][:],
                                                               func=AF.Sigmoid, bias=bg[:, ch:ch + 1], scale=1.0),
                                 [f'psa{p}', 'bg'], [f'stg{s}'])
                        c.dma('sp', gT_s[ch * 128:(ch + 1) * 128, :], stg[s][:], [f'stg{s}'], ['gT_s'], f'stg{s}')
                ws = state['w'] % 2
                state['w'] += 1
                load_w(w_in_d, C_BA, 16, dst_col=0, slot=ws)
                load_w(w_in_d, C_IW, 8, dst_col=16, slot=ws)
                for gi2, wc0 in enumerate((0, 8)):
                    for blk in range(NB):
                        p = fm_block(ws, wc0, 8, blk)
                        evac_copy(accs[0][0:8, blk * 512:(blk + 1) * 512], psa[p][0:8, :], [f'psa{p}'], ['acc0'])
                    c.dma('sp', gate_s[gi2], accs[0][0:8, :], ['acc0'], ['gate_s'], 'acc')
                p = state['ps'] % 3
                state['ps'] += 1
                for tt in range(NT):
                    for k in range(8):
                        c.op('pe', lambda e: e.matmul(psa[p][:, tt * 8:(tt + 1) * 8], lhsT=hT[:, k, tt * 128:(tt + 1) * 128],
                                                      rhs=wch[ws][:, k, 16:24], start=(k == 0), stop=(k == 7)),
                             [f'wch{ws}', f'hT{tt // 4}'], [f'psa{p}'])
                c.op('act', lambda e: e.copy(out=iw_all[:].rearrange("p t h -> p (t h)"), in_=psa[p][:, 0:NT * 8]),
                     [f'psa{p}'], ['iw_all'])
                for (col0, dst_s, fn) in ((C_ZA, za_s, AF.Silu), (C_VB, vb_s, None)):
                    for g0 in range(2):
                        ws = load_w(w_in_d, col0 + g0 * 512, 512)
                        for tt in range(NT):
                            p = state['ps'] % 3
                            state['ps'] += 1
                            for k in range(8):
                                c.op('pe', lambda e: e.matmul(psa[p][:], lhsT=hT[:, k, tt * 128:(tt + 1) * 128],
                                                              rhs=wch[ws][:, k, :], start=(k == 0), stop=(k == 7)),
                                     [f'wch{ws}', f'hT{tt // 4}'], [f'psa{p}'])
                            s = state['ts'] % 3
                            state['ts'] += 1
                            if fn is None:
                                evac_copy(tstg[s][:], psa[p][:], [f'psa{p}'], [f'tstg{s}'])
                            else:
                                c.op('act', lambda e: e.activation(out=tstg[s][:], in_=psa[p][:], func=fn),
                                     [f'psa{p}'], [f'tstg{s}'])
                            c.dma('sp', dst_s[tt * 128:(tt + 1) * 128, g0 * 512:(g0 + 1) * 512], tstg[s][:],
                                  [f'tstg{s}'], [dst_s.tensor.name], f'tstg{s}')
                for gi, (col0, dst_s) in enumerate(((C_QA, qaT_s), (C_KA, kaT_s), (C_VA, vaT_s))):
                    for g0 in range(0, 8, 4):
                        ws = load_w(w_in_d, col0 + g0 * 128, 512)
                        for cc in range(4):
                            ch = gi * 8 + g0 + cc
                            pb_ = ch % 2
                            pre, acc = pres[pb_], accs[pb_]
                            kp, ka = f'pre{pb_}', f'acc{pb_}'
                            for blk in range(NB):
                                p = fm_block(ws, cc * 128, 128, blk)
                                c.op('act', lambda e: e.copy(out=pre[:, 3 + blk * 512:3 + (blk + 1) * 512], in_=psa[p][:]),
                                     [f'psa{p}'], [kp])
                            c.op('dve', lambda e: e.tensor_scalar(out=acc[:], in0=pre[:, 0:T], scalar1=cw[:, ch, 0:1],
                                                                  scalar2=None, op0=ALU.mult), [kp, 'cw'], [ka])
                            for j in range(1, 4):
                                c.op('dve', lambda e: e.scalar_tensor_tensor(out=acc[:], in0=pre[:, j:j + T],
                                                                             scalar=cw[:, ch, j:j + 1], in1=acc[:],
                                                                             op0=ALU.mult, op1=ALU.add),
                                     [kp, 'cw', ka], [ka])
                            s = state['stg'] % 2
                            state['stg'] += 1
                            if gi == 2:
                                c.op('act', lambda e: e.activation(out=stg[s][:], in_=acc[:], func=AF.Silu),
                                     [ka], [f'stg{s}'])
                            else:
                                c.op('act', lambda e: e.activation(out=acc[:], in_=acc[:], func=AF.Silu), [ka], [ka])
                                c.op('act', lambda e: e.activation(out=sq[:], in_=acc[:], func=AF.Square), [ka], ['sq'])
                                for blk in range(NB):
                                    q = blk % 2
                                    c.op('pe', lambda e: e.matmul(psn[q][:], lhsT=ones_b[:], rhs=sq[:, blk * 512:(blk + 1) * 512],
                                                                  start=True, stop=True), ['sq', 'ones_b'], [f'psn{q}'])
                                    c.op('act', lambda e: e.activation(out=pre[:, 4 + blk * 512:4 + (blk + 1) * 512], in_=psn[q][:],
                                                                       func=AF.Ln, bias=eps_t[:], scale=1.0),
                                         [f'psn{q}', 'eps_t'], [kp])
                                c.op('act', lambda e: e.activation(out=pre[:, 4:4 + T], in_=pre[:, 4:4 + T], func=AF.Exp, scale=-0.5), [kp], [kp])
                                sc = (128.0 ** -0.5) if gi == 0 else 1.0
                                c.op('dve', lambda e: e.scalar_tensor_tensor(out=stg[s][:], in0=acc[:], scalar=sc, in1=pre[:, 4:4 + T],
                                                                             op0=ALU.mult, op1=ALU.mult),
                                     [ka, kp], [f'stg{s}'])
                            r = (g0 + cc) * 128
                            c.dma('sp', dst_s[r:r + 128, :], stg[s][:], [f'stg{s}'], [dst_s.tensor.name], f'stg{s}')
                c.barrier()
                if debug:
                    c.dma('sp', dbg['iw'], iw_all[:], ['iw_all'], [], 'dbg0')
            c.barrier()


        with ExitStack() as pro:
            ba_r = sb(pro, "ba_r", [8, T], F32)
            aa_r = sb(pro, "aa_r", [8, T], F32)
            gc_r = sb(pro, "gc_r", [8, T], F32)
            tA = sb(pro, "tA", [8, T], F32)
            tB = sb(pro, "tB", [8, T], F32)
            rmask = sb(pro, "rmask", [8, T], F32)
            alog = sb(pro, "alog", [8, 1], F32)
            dtb = sb(pro, "dtb", [8, 1], F32)
            nA = sb(pro, "nA", [8, 1], F32)
            c.dma('sp', ba_r[:], gate_s[0], ['gate_s'], ['ba_r'], 'c0')
            c.dma('sp', aa_r[:], gate_s[1], ['gate_s'], ['aa_r'], 'c1')
            c.dma('sp', alog[:], alog_d, [], ['alog'], 'c2')
            c.dma('sp', dtb[:], dtb_d, [], ['dtb'], 'c3')
            c.op('act', lambda e: e.activation(out=ba_r[:], in_=ba_r[:], func=AF.Sigmoid), ['ba_r'], ['ba_r'])
            c.op('act', lambda e: e.activation(out=aa_r[:], in_=aa_r[:], func=AF.Exp, bias=dtb[:, 0:1], scale=1.0),
                 ['aa_r', 'dtb'], ['aa_r'])
            c.op('act', lambda e: e.activation(out=aa_r[:], in_=aa_r[:], func=AF.Ln, bias=1.0, scale=1.0), ['aa_r'], ['aa_r'])
            c.op('act', lambda e: e.activation(out=nA[:], in_=alog[:], func=AF.Exp), ['alog'], ['nA'])
            c.op('dve', lambda e: e.tensor_scalar(out=nA[:], in0=nA[:], scalar1=-1.0, scalar2=None, op0=ALU.mult), ['nA'], ['nA'])
            c.op('dve', lambda e: e.tensor_scalar(out=aa_r[:], in0=aa_r[:], scalar1=nA[:, 0:1], scalar2=None, op0=ALU.mult),
                 ['aa_r', 'nA'], ['aa_r'])
            c.op('dve', lambda e: e.memset(rmask[:], 1.0), [], ['rmask'])
            c.op('dve', lambda e: e.memset(rmask[:, 0:T:64], 0.0), ['rmask'], ['rmask'])
            c.op('dve', lambda e: e.tensor_tensor_scan(out=gc_r[:], data0=rmask[:], data1=aa_r[:], initial=0.0,
                                                       op0=ALU.mult, op1=ALU.add), ['rmask', 'aa_r'], ['gc_r'])
            c.dma('sp', SRall[8:16, :], gc_r[:], ['gc_r'], ['SRall'], 'c0')
            c.op('dve', lambda e: e.tensor_scalar(out=tA[:], in0=gc_r[:], scalar1=-1.0, scalar2=None, op0=ALU.mult), ['gc_r'], ['tA'])
            c.dma('sp', SRall[0:8, :], tA[:], ['tA'], ['SRall'], 'c1')
            c.dma('sp', SRall[16:24, :], ba_r[:], ['ba_r'], ['SRall'], 'c2')
            c.op('act', lambda e: e.activation(out=tB[:], in_=gc_r[:], func=AF.Exp), ['gc_r'], ['tB'])
            c.dma('sp', SRall[40:48, :], tB[:], ['tB'], ['SRall'], 'c3')
            c.op('dve', lambda e: e.tensor_tensor(out=tA[:], in0=ba_r[:], in1=tB[:], op=ALU.mult), ['ba_r', 'tB', 'tA'], ['tA'])
            c.dma('sp', SRall[24:32, :], tA[:], ['tA'], ['SRall'], 'c1')
            gc3 = gc_r[:].rearrange("p (n c) -> p n c", c=64)
            tB3 = tB[:].rearrange("p (n c) -> p n c", c=64)
            c.op('dve', lambda e: e.tensor_tensor(out=tB3, in0=gc3[:, :, 63:64].to_broadcast([8, 64, 64]), in1=gc3,
                                                  op=ALU.subtract), ['gc_r', 'tB'], ['tB'])
            c.op('act', lambda e: e.activation(out=tB[:], in_=tB[:], func=AF.Exp), ['tB'], ['tB'])
            c.dma('sp', SRall[32:40, :], tB[:], ['tB'], ['SRall'], 'c3')
            c.barrier()
        with ExitStack() as p3:
            sel = sb(p3, "sel", [48, 32, 128], F32)
            negm = sb(p3, "negm", [128, 3, 128], BF16)
            naw_bc = sb(p3, "naw_bc", [128, 128], F32)
            c.dma('sp', sel[:], sel_d, [], ['sel'], 'c0')
            c.dma('pool', negm[:], negm_d, [], ['negm'], 'c1')
            c.dma('sp', naw_bc[:], naw_d.partition_broadcast(128), [], ['naw_bc'], 'c2')
            sets = [tuple(ps(p3, f"{nm}{si}", [128, 512], F32) for nm in ("B0_", "PG_", "PM_", "PX_")) for si in range(2)]
            B0, PG, PM, PX = sets[0]
            BTs = [sets[si][3][:, 384:512].bitcast(BF16) for si in range(2)]
            allbanks = [f"{nm}{si}" for si in range(2) for nm in ("B0_", "PG_", "PM_", "PX_")]
            hbank = [sets[si][j] for si in range(2) for j in range(4)]
            c.bank_of['B0'] = 'B0_0'
            c.bank_of['PG'] = 'PG_0'
            for si in range(2):
                for k_, b_ in (('B0a', 'B0_'), ('B0b', 'B0_'), ('B0c', 'B0_'), ('PGx', 'PG_'), ('PMx', 'PM_'),
                               ('PXa', 'PX_'), ('PXb', 'PX_'), ('PXc', 'PX_'), ('BTa', 'PX_'), ('BTb', 'PX_')):
                    c.bank_of[f'{k_}{si}'] = f'{b_}{si}'
            for h_ in range(8):
                for k_ in ('PAa', 'PAb', 'PS'):
                    c.bank_of[f'{k_}{h_}'] = allbanks[h_]
            for g in range(4):
                for j in range(8):
                    tt = g * 8 + j
                    c.op('pe', lambda e: e.transpose(out=B0[:, j * 48:(j + 1) * 48], in_=SRall[:, tt * 128:(tt + 1) * 128],
                                                     identity=ident_f[0:48, 0:48]), ['SRall', 'ident_f'], ['B0'])
                c.op('act', lambda e: e.copy(out=tokS[:, g * 8:(g + 1) * 8, :].rearrange("p t r -> p (t r)"), in_=B0[:, 0:384]),
                     ['B0'], ['tokS'])
            for h in range(8):
                c.op('pe', lambda e: e.matmul(PG[:, h * 64:(h + 1) * 64], lhsT=sel[:, 8 + h, :], rhs=SRall[:, 63:T:64],
                                              start=True, stop=True), ['sel', 'SRall'], ['PG'])
            c.op('act', lambda e: e.copy(out=eglast[:].rearrange("p h n -> p (h n)"), in_=PG[:]), ['PG'], ['eglast'])
            c.barrier()
            qin = [sb(p3, f"qin{i}", [128, 512], BF16) for i in range(2)]
            kin = [sb(p3, f"kin{i}", [128, 512], BF16) for i in range(2)]
            vin = [sb(p3, f"vin{i}", [128, 512], BF16) for i in range(2)]
            kbT = [sb(p3, f"kbT{i}", [128, 128], BF16) for i in range(2)]
            kbg = [sb(p3, f"kbg{i}", [128, 128], BF16) for i in range(2)]
            vbt = [sb(p3, f"vbt{i}", [128, 128], BF16) for i in range(2)]
            EM = [sb(p3, f"EM{i}", [128, 384], F32) for i in range(2)]
            NN = [sb(p3, f"NN{i}", [128, 256], BF16) for i in range(2)]
            PP = [sb(p3, f"PP{i}", [128, 256], BF16) for i in range(2)]
            Xs = [sb(p3, f"Xs{i}", [128, 128], BF16) for i in range(2)]
            qdT_sg = sb(p3, "qdT_sg", [128, 8, 512], BF16)
            wT_sg = sb(p3, "wT_sg", [128, 8, 512], BF16)
            QK_sg = sb(p3, "QK_sg", [128, 8, 4, 128], BF16)
            kt_sg = sb(p3, "kt_sg", [128, 8, 4, 128], BF16)
            u_sg = sb(p3, "u_sg", [128, 8, 4, 128], F32)
            o_sg = sb(p3, "o_sg", [128, 8, 4, 128], F32)
            S32 = sb(p3, "S32", [128, 8, 128], F32)
            Sb = sb(p3, "Sb", [128, 8, 128], BF16)
            vnew = sb(p3, "vnew", [128, 8, 128], BF16)
            zs = [sb(p3, f"zs{i}", [128, 1024], BF16) for i in range(2)]
            oat = [sb(p3, f"oat{i}", [128, 1024], BF16) for i in range(2)]
            t1 = sb(p3, "t1", [128, 8, 128], F32)
            ssq = sb(p3, "ssq", [128, 8], F32)
            rstd8 = sb(p3, "rstd8", [128, 8], F32)
            junk3 = sb(p3, "junk3", [128, 128], BF16)
            c.op('dve', lambda e: e.memset(S32[:], 0.0), [], [f'S32_{h}' for h in range(8)])
            c.op('dve', lambda e: e.memset(Sb[:], 0.0), [], [f'Sb_{h}' for h in range(8)])
            PPs = [[sb(p3, f"PPs{si}_{i}", [128, 256], BF16) for i in range(2)] for si in range(2)]

            def gen3a(seg, h, lt, si):
                B0, PG, PM, PX = sets[si]
                BT = BTs[si]
                b = si
                sl = (seg * 8 + h) % 2
                csl = slice(seg * 512, (seg + 1) * 512)
                if lt == 0:
                    c.dma('sp', qin[sl][:], qaT_s[h * 128:(h + 1) * 128, csl], ['qaT_s'], [f'qin{sl}'], f'qin{sl}')
                    c.dma('sp', kin[sl][:], kaT_s[h * 128:(h + 1) * 128, csl], ['kaT_s'], [f'kin{sl}'], f'kin{sl}')
                    c.dma('sp', vin[sl][:], vaT_s[h * 128:(h + 1) * 128, csl], ['vaT_s'], [f'vin{sl}'], f'vin{sl}')
                tt = seg * 4 + lt
                cs = slice(lt * 128, (lt + 1) * 128)
                gs = slice(tt * 128, (tt + 1) * 128)
                kB0a, kB0b, kB0c, kPG, kPM = f'B0a{si}', f'B0b{si}', f'B0c{si}', f'PGx{si}', f'PMx{si}'
                kPXa, kPXb, kPXc, kBTa, kBTb = f'PXa{si}', f'PXb{si}', f'PXc{si}', f'BTa{si}', f'BTb{si}'
                c.op('pe', lambda e: e.matmul(B0[:, 0:128], lhsT=sel[:, 0 + h, :], rhs=SRall[:, gs], start=True, stop=True),
                     ['sel', 'SRall'], [kB0a])
                c.op('pe', lambda e: e.matmul(B0[:, 128:256], lhsT=sel[:, 8 + h, :], rhs=SRall[:, gs], start=True, stop=True),
                     ['sel', 'SRall'], [kB0b])
                c.op('pe', lambda e: e.transpose(out=BT[:, 0:128], in_=kin[sl][:, cs], identity=ident_b[:]),
                     [f'kin{sl}', 'ident_b'], [kBTa])
                c.op('pe', lambda e: e.transpose(out=BT[:, 128:256], in_=vin[sl][:, cs], identity=ident_b[:]),
                     [f'vin{sl}', 'ident_b'], [kBTb])
                for (col, sj, mj) in ((0, 16 + h, 1), (128, 16 + h, 0), (256, 24 + h, 2)):
                    c.op('pe', lambda e: e.matmul(PM[:, col:col + 128], lhsT=sel[:, sj, :], rhs=SRall[:, gs], start=True, stop=False),
                         ['sel', 'SRall'], [kPM])
                    c.op('pe', lambda e: e.matmul(PM[:, col:col + 128], lhsT=ident_b[:], rhs=negm[:, mj, :], start=False, stop=True),
                         ['ident_b', 'negm'], [kPM])
                yield
                c.op('dve', lambda e: e.tensor_tensor(out=kbT[b][:], in0=kin[sl][:, cs], in1=B0[:, 0:128], op=ALU.mult),
                     [f'kin{sl}', kB0a], [f'kbT{b}'])
                c.op('dve', lambda e: e.tensor_tensor(out=qdT_sg[:, h, cs], in0=qin[sl][:, cs], in1=B0[:, 128:256], op=ALU.mult),
                     [f'qin{sl}', kB0b], [f'qd{h}'])
                c.op('act', lambda e: e.activation(out=EM[b][:, 0:256], in_=PM[:, 0:256], func=AF.Exp,
                                                   bias=tokS[:, tt, 0 + h:1 + h], scale=1.0), [kPM, 'tokS'], [f'EMa{b}'])
                c.op('act', lambda e: e.activation(out=EM[b][:, 256:384], in_=PM[:, 256:384], func=AF.Exp,
                                                   bias=tokS[:, tt, 8 + h:9 + h], scale=1.0), [kPM, 'tokS'], [f'EMb{b}'])
                c.op('dve', lambda e: e.tensor_scalar(out=kbg[b][:], in0=BT[:, 0:128], scalar1=tokS[:, tt, 24 + h:25 + h],
                                                      scalar2=None, op0=ALU.mult), [kBTa, 'tokS'], [f'kbg{b}'])
                c.op('dve', lambda e: e.tensor_scalar(out=kt_sg[:, h, lt, :], in0=BT[:, 0:128], scalar1=tokS[:, tt, 32 + h:33 + h],
                                                      scalar2=None, op0=ALU.mult), [kBTa, 'tokS'], [f'kt{h}'])
                c.op('act', lambda e: e.activation(out=vbt[b][:], in_=BT[:, 128:256], func=AF.Copy,
                                                   scale=tokS[:, tt, 16 + h:17 + h]), [kBTb, 'tokS'], [f'vbt{b}'])
                yield
                c.op('pe', lambda e: e.matmul(PG[:, 0:128], lhsT=kin[sl][:, cs], rhs=qin[sl][:, cs], start=True, stop=True),
                     [f'kin{sl}', f'qin{sl}'], [kPG])
                c.op('pe', lambda e: e.matmul(PG[:, 128:256], lhsT=kin[sl][:, cs], rhs=kbT[b][:], start=True, stop=True),
                     [f'kin{sl}', f'kbT{b}'], [kPG])
                c.op('pe', lambda e: e.matmul(PG[:, 256:384], lhsT=kbT[b][:], rhs=kin[sl][:, cs], start=True, stop=True),
                     [f'kin{sl}', f'kbT{b}'], [kPG])
                yield
                c.op('dve', lambda e: e.tensor_tensor(out=NN[b][:], in0=PG[:, 128:384], in1=EM[b][:, 128:384], op=ALU.mult),
                     [kPG, f'EMa{b}', f'EMb{b}'], [f'NN{b}'])
                c.op('dve', lambda e: e.tensor_tensor(out=QK_sg[:, h, lt, :], in0=PG[:, 0:128], in1=EM[b][:, 0:128], op=ALU.mult),
                     [kPG, f'EMa{b}'], [f'QK{h}'])
                X = Xs[b]
                c.op('dve', lambda e: e.tensor_tensor(out=X[:], in0=ident_b[:], in1=NN[b][:, 0:128], op=ALU.subtract),
                     ['ident_b', f'NN{b}'], [f'X{b}'])
                yield
                Pk, Ptk, pkey = NN[b][:, 0:128], NN[b][:, 128:256], f'NN{b}'

                def sq(k, Pk, Ptk, pkey):
                    c.op('pe', lambda e: e.matmul(B0[:, 256:384], lhsT=Pk, rhs=Ptk, start=True, stop=True), [pkey], [kB0c])
                    if k < 5:
                        c.op('pe', lambda e: e.matmul(B0[:, 384:512], lhsT=Ptk, rhs=Pk, start=True, stop=True), [pkey], [kB0c])
                sq(1, Pk, Ptk, pkey)
                yield
                for k in range(1, 6):
                    pp = PPs[si][k % 2]
                    wd = 256 if k < 5 else 128
                    c.op('act', lambda e: e.copy(out=pp[:, 0:wd], in_=B0[:, 256:256 + wd]), [kB0c], [f'PP{si}_{k % 2}'])
                    Ptk, Pk, pkey = pp[:, 0:128], pp[:, 128:256], f'PP{si}_{k % 2}'
                    yield
                    if k < 5:
                        sq(k + 1, Pk, Ptk, pkey)
                    c.op('pe', lambda e: e.matmul(PX[:, 0:128], lhsT=Ptk, rhs=X[:], start=True, stop=True), [pkey, f'X{b}'], [kPXa])
                    yield
                    c.op('dve', lambda e: e.tensor_tensor(out=X[:], in0=X[:], in1=PX[:, 0:128], op=ALU.add),
                         [f'X{b}', kPXa], [f'X{b}'])
                yield
                c.op('pe', lambda e: e.matmul(PX[:, 128:256], lhsT=X[:], rhs=vbt[b][:], start=True, stop=True),
                     [f'X{b}', f'vbt{b}'], [kPXb])
                c.op('pe', lambda e: e.matmul(PX[:, 256:384], lhsT=kbg[b][:], rhs=X[:], start=True, stop=True),
                     [f'X{b}', f'kbg{b}'], [kPXc])
                yield
                c.op('act', lambda e: e.copy(out=u_sg[:, h, lt, :], in_=PX[:, 128:256]), [kPXb], [f'u{h}'])
                c.op('dve', lambda e: e.tensor_copy(out=wT_sg[:, h, cs], in_=PX[:, 256:384]), [kPXc], [f'wT{h}'])
                yield

            def run_pipelined(jobs, width=2):
                live = {}
                jobs = list(jobs)
                nxt = 0
                while nxt < len(jobs) or live:
                    for si in range(width):
                        if si not in live and nxt < len(jobs):
                            live[si] = jobs[nxt](si)
                            nxt += 1
                    for si in list(live):
                        try:
                            next(live[si])
                        except StopIteration:
                            del live[si]

            for seg in range(8):
                run_pipelined([(lambda si, seg=seg, h=h, lt=lt: gen3a(seg, h, lt, si)) for h in range(8) for lt in range(4)])
                for hg in range(1):
                    heads = range(8)
                    for nl in range(8):
                        n = seg * 8 + nl
                        lt, hf = nl // 2, nl % 2
                        rows = slice(64 * hf, 64 * hf + 64)
                        cols = slice(lt * 128 + 64 * hf, lt * 128 + 64 * hf + 64)
                        for h in heads:
                            pa = hbank[h]
                            c.op('pe', lambda e: e.matmul(pa[rows, 0:128], lhsT=wT_sg[:, h, cols], rhs=Sb[:, h, :], start=True, stop=True),
                                 [f'wT{h}', f'Sb_{h}'], [f'PAa{h}'])
                        for h in heads:
                            pa = hbank[h]
                            c.op('dve', lambda e: e.tensor_tensor(out=vnew[rows, h, :], in0=u_sg[rows, h, lt, :], in1=pa[rows, 0:128],
                                                                  op=ALU.subtract), [f'u{h}', f'PAa{h}'], [f'vn{h}'])
                        for h in heads:
                            pa = hbank[h]
                            c.op('pe', lambda e: e.matmul(pa[rows, 128:256], lhsT=qdT_sg[:, h, cols], rhs=Sb[:, h, :], start=True, stop=False),
                                 [f'qd{h}', f'Sb_{h}'], [f'PAb{h}'])
                            c.op('pe', lambda e: e.matmul(pa[rows, 128:256], lhsT=QK_sg[rows, h, lt, 64 * hf:64 * hf + 64], rhs=vnew[rows, h, :],
                                                          start=False, stop=True), [f'QK{h}', f'vn{h}'], [f'PAb{h}'])
                            c.op('pe', lambda e: e.matmul(pa[:, 256:384], lhsT=kt_sg[rows, h, lt, :], rhs=vnew[rows, h, :],
                                                          start=True, stop=True), [f'kt{h}', f'vn{h}'], [f'PS{h}'])
                        for h in heads:
                            pa = hbank[h]
                            c.op('dve', lambda e: e.scalar_tensor_tensor(out=S32[:, h, :], in0=S32[:, h, :], scalar=eglast[:, h, n:n + 1],
                                                                         in1=pa[:, 256:384], op0=ALU.mult, op1=ALU.add),
                                 [f'S32_{h}', 'eglast', f'PS{h}'], [f'S32_{h}'])
                            c.op('act', lambda e: e.copy(out=o_sg[rows, h, lt, :], in_=pa[rows, 128:256]), [f'PAb{h}'], [f'o{h}'])
                            c.op('act', lambda e: e.copy(out=Sb[:, h, :], in_=S32[:, h, :]), [f'S32_{h}'], [f'Sb_{h}'])
                for lt in range(4):
                    tt = seg * 4 + lt
                    zb = tt % 2
                    c.dma('sp', zs[zb][:], za_s[tt * 128:(tt + 1) * 128, :], ['za_s'], [f'zs{zb}'], f'zs{zb}')
                    for h in range(8):
                        c.op('act', lambda e: e.activation(out=junk3[:], in_=o_sg[:, h, lt, :], func=AF.Square, accum_out=ssq[:, h:h + 1]),
                             [f'o{h}'], [f'ssq{h}', 'junk3'])
                    allssq = [f'ssq{h}' for h in range(8)]
                    c.op('pool', lambda e: e.tensor_scalar(out=rstd8[:], in0=ssq[:], scalar1=1.0 / 128, scalar2=EPS, op0=ALU.mult, op1=ALU.add),
                         allssq, ['rstd8'])
                    c.op('pool', lambda e: e.tensor_tensor(out=rstd8[:], in0=rstd8[:], in1=mhalf_t[:].to_broadcast([128, 8]), op=ALU.pow),
                         ['rstd8', 'mhalf_t'], ['rstd8'])
                    c.op('dve', lambda e: e.tensor_tensor(out=t1[:], in0=o_sg[:, :, lt, :], in1=rstd8[:].unsqueeze(2).to_broadcast([128, 8, 128]),
                                                          op=ALU.mult), [f'o{h}' for h in range(8)] + ['rstd8'], ['t1'])
                    c.op('dve', lambda e: e.tensor_tensor(out=t1[:], in0=t1[:], in1=naw_bc[:].unsqueeze(1).to_broadcast([128, 8, 128]),
                                                          op=ALU.mult), ['t1', 'naw_bc'], ['t1'])
                    c.op('dve', lambda e: e.tensor_tensor(out=oat[zb][:], in0=t1[:].rearrange("p h e -> p (h e)"), in1=zs[zb][:], op=ALU.mult),
                         ['t1', f'zs{zb}'], [f'oat{zb}'])
                    c.dma('sp', oa_s[tt * 128:(tt + 1) * 128, :], oat[zb][:], [f'oat{zb}'], ['oa_s'], f'oat{zb}')
            c.barrier()


        gdn_top.close()
        NIT = 14
        SCALE_B = 128.0 ** -0.5
        with ExitStack() as p4:
            relt = sb(p4, "relt", [32, 8], F32)
            Jm = sb(p4, "Jm", [128, 128], F32)
            visneg_b = sb(p4, "visneg_b", [128, 128], BF16)
            cb = sb(p4, "cb", [128, 8], F32)
            G = sb(p4, "G", [128, 8, 1024], BF16)
            ikT = sb(p4, "ikT", [128, T], BF16)
            scores = sb(p4, "scores", [128, T], F32)
            Hk = scores[:, 0:1024]
            fr_sb = scores[0:8, 1024:2176]
            oh = scores[0:32, 2176:3328]
            mk = sb(p4, "mk", [128, T], BF16)
            maskT = sb(p4, "maskT", [128, 32, 512], BF16)
            iqb = [sb(p4, f"iqb{i}", [128, 4, 512], BF16) for i in range(2)]
            KTh = [sb(p4, f"KTh{i}", [128, T], BF16) for i in range(2)]
            Vh = [sb(p4, f"Vh{i}", [128, 32, 130], BF16) for i in range(2)]
            QTh = [sb(p4, f"QTh{i}", [128, 512], BF16) for i in range(2)]
            PTe = [sb(p4, f"PTe{i}", [128, 512], BF16) for i in range(2)]
            PTm = [sb(p4, f"PTm{i}", [128, 512], BF16) for i in range(2)]
            ME = [sb(p4, f"ME{i}", [128, 512], BF16) for i in range(2)]
            obuf = sb(p4, "obuf", [128, 4, 1024], BF16)
            sm = sb(p4, "sm", [128, 8], F32)
            wfs = sb(p4, "wfs", [128, 16], F32)
            pw_bc = sb(p4, "pw_bc", [128, 16], F32)
            mone_t = sb(p4, "mone_t", [128, 1], F32)
            pos = [sb(p4, f"pos{i}", [128, 260], F32) for i in range(2)]
            rden2 = sb(p4, "rden2", [128, 2, 2], F32)
            rden = sb(p4, "rden", [128, 4], F32)
            PSC = [ps(p4, f"PSC{i}", [128, 512], F32) for i in range(2)]
            PL = [ps(p4, f"PL{i}", [128, 512], F32) for i in range(2)]
            PO = [ps(p4, f"PO{i}", [128, 512], F32) for i in range(2)]
            BTm = ps(p4, "BTm", [128, 1024], BF16)
            PF = ps(p4, "PF", [128, 512], F32)
            c.dma('sp', relt[:], rel_d, [], ['relt'], 'c0')
            c.dma('sp', oh, oh_d, [], ['oh'], 'c1')
            c.dma('sp', Jm[:], J_d, [], ['Jm'], 'c2')
            c.dma('pool', visneg_b[:], visneg_d, [], ['visneg_b'], 'c3')
            c.dma('sp', pw_bc[:], pw_d, [], ['pw_bc'], 'c5')
            c.op('dve', lambda e: e.memset(mone_t[:], -1.0), [], ['mone_t'])
            c.dma('sp', cb[:], rel_d[15:16, :].partition_broadcast(128), [], ['cb'], 'c4')
            c.dma('sp', ikT[:], ikT_s, ['ikT_s'], ['ikT'], 'c0')
            for i in range(2):
                c.op('dve', lambda e: e.memset(Vh[i][:, :, 128:130], 1.0), [], [f'Vh{i}'])
            for j in range(3):
                c.op('pe', lambda e: e.matmul(PF[0:8, 0:384], lhsT=relt[:], rhs=oh[:, j * 384:(j + 1) * 384], start=True, stop=True),
                     ['relt', 'oh'], ['PF'])
                c.op('act', lambda e: e.activation(out=fr_sb[:, j * 384:(j + 1) * 384], in_=PF[0:8, 0:384], func=AF.Exp), ['PF'], ['fr_sb'])
            c.dma('sp', fr_s, fr_sb, ['fr_sb'], ['fr_s'], 'c1')
            for h in range(8):
                hank = bass.AP(tensor=fr_s.tensor, offset=h * 1152, ap=[[1, 128], [1, 1024]])
                c.dma('sp', Hk, hank, ['fr_s'], ['Hk'], 'c2')
                for half in range(2):
                    c.op('pe', lambda e: e.matmul(PF[:, :], lhsT=Jm[:], rhs=Hk[:, half * 512:(half + 1) * 512], start=True, stop=True),
                         ['Jm', 'Hk'], ['PF'])
                    c.op('act', lambda e: e.copy(out=G[:, h, half * 512:(half + 1) * 512], in_=PF[:, :]), ['PF'], ['G'])
            c.barrier()
            dgs = [sb(p4, f"dgs{i}", [128, 8, 128], BF16) for i in range(2)]
            rhb = [sb(p4, f"rhb{i}", [128, 512], BF16) for i in range(4)]
            maskT2 = [maskT, sb(p4, "maskT_b", [128, 32, 512], BF16)]

            scores2 = [scores, sb(p4, "scores_b", [128, T], F32)]

            def scoring(qs, qi):
                ib = qs % 2
                qt = 4 * qs + qi
                n = 128 * (qt + 1)
                sc_ = scores2[qt % 2]
                skey = f'scores{qt % 2}'
                dg = dgs[qt % 2]
                for h in range(8):
                    c.op('act', lambda e: e.activation(out=dg[:, h, :], in_=ident_b[:], func=AF.Copy, scale=iw_all[:, qt, h:h + 1]),
                         ['ident_b', 'iw_all'], [f'dgs{qt % 2}'])
                cnt_r = [0]
                for kb in range((n + 511) // 512):
                    w = min(512, n - kb * 512)

                    def smm(h):
                        j, half = h // 2, h % 2
                        rows = slice(64 * half, 64 * half + 64)
                        p = h % 2
                        c.op('pe', lambda e: e.matmul(PSC[p][:, 0:w], lhsT=iqb[ib][rows, j, qi * 128:(qi + 1) * 128],
                                                      rhs=ikT[rows, kb * 512:kb * 512 + w], start=True, stop=True),
                             [f'iqb{ib}', 'ikT'], [f'PSC{p}'])
                        r = cnt_r[0] % 4
                        cnt_r[0] += 1
                        c.op('act', lambda e: e.activation(out=rhb[r][:, 0:w], in_=PSC[p][:, 0:w], func=AF.Relu), [f'PSC{p}'], [f'rhb{r}'])
                        return r

                    lastkb = (kb == (n + 511) // 512 - 1)

                    def amm(h, r):
                        c.op('pe', lambda e: e.matmul(PF[:, 0:w], lhsT=dg[:, h, :], rhs=rhb[r][:, 0:w], start=(h == 0), stop=(h == 7 and not lastkb)),
                             [f'dgs{qt % 2}', f'rhb{r}'], ['PF'])
                        if h == 7 and lastkb:
                            c.op('pe', lambda e: e.matmul(PF[:, w - 128:w], lhsT=ident_b[:], rhs=visneg_b[:], start=False, stop=True),
                                 ['ident_b', 'visneg_b'], ['PF'])
                    rr = [None] * 8
                    rr[0] = smm(0)
                    rr[1] = smm(1)
                    for h in range(8):
                        if h + 2 < 8:
                            rr[h + 2] = smm(h + 2)
                        amm(h, rr[h])
                        yield 1.1
                    c.op('act', lambda e: e.copy(out=sc_[:, kb * 512:kb * 512 + w], in_=PF[:, 0:w]), ['PF'], [skey])

            def select(qs, qi):
                qt = 4 * qs + qi
                n = 128 * (qt + 1)
                mT_ = maskT2[qs % 2]
                mkey = f'maskT{qs % 2}'
                sc_ = scores2[qt % 2]
                skey = f'scores{qt % 2}'
                if qt >= 2:
                    c.op('dve', lambda e: e.tensor_reduce(out=sm[:, 0:1], in_=sc_[:, 0:n], op=ALU.max, axis=AX.X), [skey], ['hi'])
                    c.op('dve', lambda e: e.tensor_reduce(out=sm[:, 1:2], in_=sc_[:, 0:n - 128], op=ALU.min, axis=AX.X), [skey], ['lo'])
                    c.op('dve', lambda e: e.tensor_tensor(out=sm[:, 2:3], in0=sm[:, 0:1], in1=sm[:, 1:2], op=ALU.subtract), ['hi', 'lo'], ['w0'])
                    c.op('dve', lambda e: e.tensor_scalar(out=wfs[:, 0:NIT], in0=pw_bc[:, 0:NIT], scalar1=sm[:, 2:3], scalar2=None, op0=ALU.mult),
                         ['w0', 'pw_bc'], ['wfs'])
                    c.op('dve', lambda e: e.tensor_tensor(out=sm[:, 3:4], in0=sm[:, 1:2], in1=wfs[:, 0:1], op=ALU.add), ['lo', 'wfs'], ['mid'])
                    yield 2.0 * n / 1000.0 + 2.0
                    for it in range(1, NIT + 1):
                        c.op('dve', lambda e: e.tensor_scalar(out=mk[:, 0:n], in0=sc_[:, 0:n], scalar1=sm[:, 3:4], scalar2=None,
                                                              op0=ALU.is_ge, op1=ALU.add, accum_out=sm[:, 4:5]),
                             [skey, 'mid'], ['mk', 'cnt'])
                        last = (it == NIT)
                        c.op('dve', lambda e: e.tensor_scalar(out=sm[:, 5:6], in0=sm[:, 4:5], scalar1=255.5, scalar2=(1.0 if last else 0.5),
                                                              op0=ALU.is_ge, op1=ALU.subtract), ['cnt'], ['ge'])
                        dst, dkey = (sm[:, 1:2], 'lo') if last else (sm[:, 3:4], 'mid')
                        c.op('dve', lambda e: e.scalar_tensor_tensor(out=dst, in0=sm[:, 5:6], scalar=wfs[:, it - 1:it], in1=sm[:, 3:4],
                                                                     op0=ALU.mult, op1=ALU.add), ['ge', 'wfs', 'mid'], [dkey])
                        yield n / 1000.0 + 1.0
                    c.op('dve', lambda e: e.tensor_scalar(out=mk[:, 0:n], in0=sc_[:, 0:n], scalar1=sm[:, 1:2], scalar2=None,
                                                          op0=ALU.is_ge), [skey, 'lo'], ['mk'])
                else:
                    c.op('dve', lambda e: e.tensor_scalar(out=mk[:, 0:n], in0=sc_[:, 0:n], scalar1=-1e4, scalar2=None,
                                                          op0=ALU.is_ge), [skey], ['mk'])
                for k0 in range(0, qt + 1, 4):
                    kn = min(4, qt + 1 - k0)
                    for a in range(kn):
                        c.op('pe', lambda e: e.transpose(out=BTm[:, a * 128:(a + 1) * 128], in_=mk[:, (k0 + a) * 128:(k0 + a + 1) * 128],
                                                         identity=ident_b[:]), ['mk', 'ident_b'], ['BTm'])
                    c.op('act', lambda e: e.copy(out=mT_[:, k0:k0 + kn, qi * 128:(qi + 1) * 128],
                                                 in_=BTm[:, 0:kn * 128].rearrange("p (k t) -> p k t", t=128)), ['BTm'], [mkey])
                    yield 1.0

            def gen_attn(qs):
                mT_ = maskT2[qs % 2]
                mkey = f'maskT{qs % 2}'
                nk = 4 * qs + 4
                for h in range(8):
                    hb = (qs * 8 + h) % 2
                    c.dma('sp', KTh[hb][:, 0:nk * 128], kbT_s[h * 128:(h + 1) * 128, 0:nk * 128], ['kbT_s'], [f'KTh{hb}'], f'KTh{hb}')
                    c.dma('sp', Vh[hb][:, 0:nk, 0:128], vb_s.rearrange("(t p) c -> p t c", p=128)[:, 0:nk, h * 128:(h + 1) * 128],
                          ['vb_s'], [f'Vh{hb}'], f'Vh{hb}')
                    c.dma('sp', QTh[hb][:], qbT_s[h * 128:(h + 1) * 128, qs * 512:(qs + 1) * 512], ['qbT_s'], [f'QTh{hb}'], f'QTh{hb}')
                    started = [False, False]

                    def logits(kt):
                        p = kt % 2
                        c.op('pe', lambda e: e.matmul(PL[p][:, :], lhsT=KTh[hb][:, kt * 128:(kt + 1) * 128], rhs=QTh[hb][:, :], start=True, stop=True),
                             [f'KTh{hb}', f'QTh{hb}'], [f'PL{p}'])
                    logits(0)
                    if nk > 1:
                        logits(1)
                    for kt in range(nk):
                        p = kt % 2
                        near = kt >= 4 * qs - 1
                        if near:
                            j = kt - (4 * qs - 1)
                            c.op('act', lambda e: e.activation(out=PTe[p][:], in_=PL[p][:, :], func=AF.Exp, scale=SCALE_B), [f'PL{p}'], [f'PTe{p}'])
                            c.op('pool', lambda e: e.tensor_tensor(out=ME[p][:], in0=mT_[:, kt, :], in1=G[:, h, 512 - 128 * j:1024 - 128 * j], op=ALU.mult),
                                 [mkey, 'G'], [f'ME{p}'])
                            c.op('pool', lambda e: e.tensor_tensor(out=PTm[p][:], in0=PTe[p][:], in1=ME[p][:], op=ALU.mult),
                                 [f'PTe{p}', f'ME{p}'], [f'PTm{p}'])
                        else:
                            c.op('act', lambda e: e.activation(out=PTe[p][:], in_=PL[p][:, :], func=AF.Exp, scale=SCALE_B, bias=cb[:, h:h + 1]),
                                 [f'PL{p}', 'cb'], [f'PTe{p}'])
                            c.op('pool', lambda e: e.tensor_tensor(out=PTm[p][:], in0=PTe[p][:], in1=mT_[:, kt, :], op=ALU.mult),
                                 [f'PTe{p}', mkey], [f'PTm{p}'])
                        if kt + 2 < nk:
                            logits(kt + 2)
                        for ti in range(4):
                            if kt > 4 * qs + ti:
                                continue
                            bk = ti // 2
                            c0 = (ti % 2) * 130
                            st = not started[bk]
                            started[bk] = True
                            c.op('pe', lambda e: e.matmul(PO[bk][:, c0:c0 + 129], lhsT=PTm[p][:, ti * 128:(ti + 1) * 128], rhs=Vh[hb][:, kt, 0:129],
                                                          start=st, stop=(kt == 4 * qs + ti), skip_group_check=True), [f'PTm{p}', f'Vh{hb}'], [f'PO{bk}'])
                        yield 1.25
                    for bk in range(2):
                        c.op('act', lambda e: e.copy(out=pos[bk][:], in_=PO[bk][:, 0:260]), [f'PO{bk}'], [f'pos{bk}'])
                        c.op('pool', lambda e: e.tensor_tensor(out=rden2[:, bk, :], in0=pos[bk][:, 128:259:130], in1=mone_t[:].to_broadcast([128, 2]),
                                                               op=ALU.pow), [f'pos{bk}', 'mone_t'], [f'rden2_{bk}'])
                        for t2 in range(2):
                            ti = bk * 2 + t2
                            c.op('pool', lambda e: e.tensor_scalar(out=obuf[:, ti, h * 128:(h + 1) * 128], in0=pos[bk][:, t2 * 130:t2 * 130 + 128],
                                                                   scalar1=rden2[:, bk, t2:t2 + 1], scalar2=None, op0=ALU.mult),
                                 [f'pos{bk}', f'rden2_{bk}'], ['obuf'])
                    yield 3.0
                c.dma('sp', ob_s[qs * 512:(qs + 1) * 512, :].rearrange("(ti p) c -> p ti c", p=128), obuf[:], ['obuf'], ['ob_s'], 'obuf')

            done_S, done_T, done_A = set(), set(), set()

            def stream_S():
                for g in range(32):
                    qs, qi = g // 4, g % 4
                    if g >= 2:
                        yield ('wait', lambda g=g: (g - 2) in done_T)
                    if qi == 0:
                        ib = qs % 2
                        c.dma('sp', iqb[ib][:], iqT_s.rearrange("(j r) t -> r j t", r=128)[:, :, qs * 512:(qs + 1) * 512],
                              ['iqT_s'], [f'iqb{ib}'], f'iqb{ib}')
                    yield from scoring(qs, qi)
                    done_S.add(g)

            def stream_T():
                for g in range(32):
                    qs, qi = g // 4, g % 4
                    yield ('wait', lambda g=g: g in done_S)
                    if qi == 0:
                        if qs >= 2:
                            yield ('wait', lambda qs=qs: (qs - 2) in done_A)
                        mT_ = maskT2[qs % 2]
                        for q3 in range(3):
                            c.op('dve', lambda e: e.memset(mT_[:, 4 * qs + q3 + 1:4 * qs + 4, q3 * 128:(q3 + 1) * 128], 0.0), [], [f'maskT{qs % 2}'])
                    yield from select(qs, qi)
                    done_T.add(g)

            def stream_A():
                for qs in range(8):
                    yield ('wait', lambda qs=qs: (4 * qs + 3) in done_T)
                    yield from gen_attn(qs)
                    done_A.add(qs)

            def run_streams(gens):
                st = [{'g': g, 't': 0.0, 'blk': None} for g in gens]
                while st:
                    ready = []
                    for x in st:
                        if x['blk'] is not None and x['blk']():
                            x['blk'] = None
                            others = [y['t'] for y in st if y is not x and y['blk'] is None]
                            if others:
                                x['t'] = max(x['t'], min(others))
                        if x['blk'] is None:
                            ready.append(x)
                    assert ready, "stream deadlock"
                    x = min(ready, key=lambda y: y['t'])
                    try:
                        r = next(x['g'])
                    except StopIteration:
                        st.remove(x)
                        continue
                    if isinstance(r, tuple):
                        if not r[1]():
                            x['blk'] = r[1]
                    else:
                        x['t'] += float(r or 1.0)
            run_streams([stream_S(), stream_T(), stream_A()])
            c.barrier()

        w_pre = ExitStack()
        W1 = sb(w_pre, "W1", [128, 8, 4096], BF16)
        with ExitStack() as p5:
            Wa = sb(p5, "Wa", [128, 8, 1024], BF16)
            Wb = sb(p5, "Wb", [128, 8, 1024], BF16)
            Wo = sb(p5, "Wo", [128, 8, 1024], BF16)
            n2w_bc = sb(p5, "n2w_bc", [128, D], F32)
            for wi, (wt, wd) in enumerate(((Wa, wpa_d), (Wb, wpb_d), (Wo, wo_d))):
                for hf in range(2):
                    c.dma('pool', wt[:, :, hf * 512:(hf + 1) * 512], wd.rearrange("(k p) c -> p k c", p=128)[:, :, hf * 512:(hf + 1) * 512],
                          [], [f'W5_{wi}_{hf}'], f'c{wi * 2 + hf}')
            c.dma('sp', n2w_bc[:], n2w_d.partition_broadcast(128), [], ['n2w_bc'], 'c6')
            for j in range(8):
                c.dma('pool', W1[:, :, j * 512:(j + 1) * 512], wf1_d.rearrange("(k p) f -> p k f", p=128)[:, :, j * 512:(j + 1) * 512],
                      [], [f'W1_{j}'], f'w1_{j % 4}')
            oin = [sb(p5, f"oin{i}", [128, 1024], BF16) for i in range(2)]
            oaT = sb(p5, "oaT", [128, 8, 512], BF16)
            obT = sb(p5, "obT", [128, 8, 512], BF16)
            g0 = sb(p5, "g0", [128, 8, 512], BF16)
            g1 = sb(p5, "g1", [128, 8, 512], BF16)
            mT = sb(p5, "mT", [128, 8, 512], BF16)
            t1f = [sb(p5, f"t1f{i}", [128, 512], F32) for i in range(2)]
            t2f = [sb(p5, f"t2f{i}", [128, 512], F32) for i in range(2)]
            xin = [sb(p5, f"xin{i}", [128, D], F32) for i in range(2)]
            x1t = [sb(p5, f"x1t{i}", [128, D], F32) for i in range(2)]
            h2b = [sb(p5, f"h2b{i}", [128, D], BF16) for i in range(2)]
            h2T = [sb(p5, f"h2T{i}", [128, 8, 128], BF16) for i in range(2)]
            junk5 = sb(p5, "junk5", [128, D], BF16)
            ss5 = sb(p5, "ss5", [128, NT], F32)
            rs5 = sb(p5, "rs5", [128, NT], F32)
            ptr = [ps(p5, f"ptr{i}", [128, 8, 128], BF16) for i in range(2)]
            ppas = [ps(p5, f"ppa{i}", [128, 512], F32) for i in range(2)]
            ppbs = [ps(p5, f"ppb{i}", [128, 512], F32) for i in range(2)]
            pout = [ps(p5, f"pout{i}", [128, 512], F32) for i in range(2)]
            nti = 0
            for blk in range(NB):
                csl = slice(blk * 512, (blk + 1) * 512)
                c.dma('sp', g0[:], gT_s[0:1024, csl].rearrange("(c p) t -> p c t", p=128), ['gT_s'], ['g0'], 'g0')
                c.dma('sp', g1[:], gT_s[1024:2048, csl].rearrange("(c p) t -> p c t", p=128), ['gT_s'], ['g1'], 'g1')
                for (src_s, dstT, key) in ((oa_s, oaT, 'oaT'), (ob_s, obT, 'obT')):
                    for ti in range(4):
                        tt = blk * 4 + ti
                        b = nti % 2
                        nti += 1
                        c.dma('sp', oin[b][:], src_s[tt * 128:(tt + 1) * 128, :], [src_s.tensor.name], [f'oin{b}'], f'oin{b}')
                        for k in range(8):
                            c.op('pe', lambda e: e.transpose(out=ptr[b][:, k, :], in_=oin[b][:, k * 128:(k + 1) * 128], identity=ident_b[:]),
                                 [f'oin{b}', 'ident_b'], [f'ptr{b}'])
                        evq = 'act' if nti % 2 == 0 else 'dve'
                        if evq == 'act':
                            c.op('act', lambda e: e.copy(out=dstT[:, :, ti * 128:(ti + 1) * 128], in_=ptr[b][:]), [f'ptr{b}'], [key])
                        else:
                            c.op('dve', lambda e: e.tensor_copy(out=dstT[:, :, ti * 128:(ti + 1) * 128], in_=ptr[b][:]), [f'ptr{b}'], [key])
                for dc in range(8):
                    tb = dc % 2
                    ppa, ppb = ppas[tb], ppbs[tb]
                    for k in range(8):
                        c.op('pe', lambda e: e.matmul(ppa[:, :], lhsT=Wa[:, k, dc * 128:(dc + 1) * 128], rhs=oaT[:, k, :], start=(k == 0), stop=(k == 7)),
                             [f'W5_0_{dc // 4}', 'oaT'], [f'ppa{tb}'])
                    for k in range(8):
                        c.op('pe', lambda e: e.matmul(ppb[:, :], lhsT=Wb[:, k, dc * 128:(dc + 1) * 128], rhs=obT[:, k, :], start=(k == 0), stop=(k == 7)),
                             [f'W5_1_{dc // 4}', 'obT'], [f'ppb{tb}'])
                    c.op('dve', lambda e: e.tensor_tensor(out=t1f[tb][:], in0=ppa[:, :], in1=g0[:, dc, :], op=ALU.mult), [f'ppa{tb}', 'g0'], [f't1f{tb}'])
                    c.op('dve', lambda e: e.tensor_tensor(out=t2f[tb][:], in0=ppb[:, :], in1=g1[:, dc, :], op=ALU.mult), [f'ppb{tb}', 'g1'], [f't2f{tb}'])
                    c.op('pool', lambda e: e.tensor_tensor(out=mT[:, dc, :], in0=t1f[tb][:], in1=t2f[tb][:], op=ALU.add),
                         [f't1f{tb}', f't2f{tb}'], ['mT'])
                for ti in range(4):
                    tt = blk * 4 + ti
                    xb = tt % 2
                    c.dma('sp', xin[xb][:], x_d[tt * 128:(tt + 1) * 128, :], [], [f'xin{xb}'], f'xin{xb}')
                    for hf in range(2):
                        for k in range(8):
                            c.op('pe', lambda e: e.matmul(pout[hf][:, :], lhsT=mT[:, k, ti * 128:(ti + 1) * 128], rhs=Wo[:, k, hf * 512:(hf + 1) * 512],
                                                          start=(k == 0), stop=(k == 7)), ['mT', f'W5_2_{hf}'], [f'pout{hf}'])
                        c.op('dve', lambda e: e.tensor_tensor(out=x1t[xb][:, hf * 512:(hf + 1) * 512], in0=pout[hf][:, :],
                                                              in1=xin[xb][:, hf * 512:(hf + 1) * 512], op=ALU.add),
                             [f'pout{hf}', f'xin{xb}'], [f'x1t{xb}'])
                    c.dma('sp', x1_s[tt * 128:(tt + 1) * 128, :], x1t[xb][:], [f'x1t{xb}'], ['x1_s'], f'x1t{xb}')
                    c.op('act', lambda e: e.activation(out=junk5[:], in_=x1t[xb][:], func=AF.Square, accum_out=ss5[:, tt:tt + 1]),
                         [f'x1t{xb}'], [f'ss5_{tt}', 'junk5'])
                    c.op('pool', lambda e: e.tensor_scalar(out=rs5[:, tt:tt + 1], in0=ss5[:, tt:tt + 1], scalar1=1.0 / D, scalar2=EPS,
                                                           op0=ALU.mult, op1=ALU.add), [f'ss5_{tt}'], [f'rs5_{tt}'])
                    c.op('pool', lambda e: e.tensor_tensor(out=rs5[:, tt:tt + 1], in0=rs5[:, tt:tt + 1], in1=mhalf_t[:], op=ALU.pow),
                         [f'rs5_{tt}', 'mhalf_t'], [f'rs5_{tt}'])
                    c.op('dve', lambda e: e.scalar_tensor_tensor(out=h2b[xb][:], in0=x1t[xb][:], scalar=rs5[:, tt:tt + 1], in1=n2w_bc[:],
                                                                 op0=ALU.mult, op1=ALU.mult), [f'x1t{xb}', f'rs5_{tt}', 'n2w_bc'], [f'h2b{xb}'])
                    for k in range(8):
                        c.op('pe', lambda e: e.transpose(out=ptr[xb][:, k, :], in_=h2b[xb][:, k * 128:(k + 1) * 128], identity=ident_b[:]),
                             [f'h2b{xb}', 'ident_b'], [f'ptr{xb}'])
                    c.op('act', lambda e: e.copy(out=h2T[xb][:], in_=ptr[xb][:]), [f'ptr{xb}'], [f'h2T{xb}'])
                    c.dma('sp', h2T_s[:, :, tt * 128:(tt + 1) * 128], h2T[xb][:], [f'h2T{xb}'], ['h2T_s'], f'h2T{xb}')
            c.barrier()

        with ExitStack() as p6:
            W2 = sb(p6, "W2", [128, 32, 1024], BF16)
            nfw_bc = sb(p6, "nfw_bc", [128, D], F32)
            for j in range(8):
                c.dma('pool', W2[:, j * 4:(j + 1) * 4, :], wf2_d.rearrange("(cc p) d -> p cc d", p=128)[:, j * 4:(j + 1) * 4, :],
                      [], [f'W2_{j}'], f'c{4 + j % 4}')
            c.dma('sp', nfw_bc[:], nfw_d.partition_broadcast(128), [], ['nfw_bc'], 'c8')
            TB = 256
            hin = [sb(p6, f"hin{i}", [128, 8, TB], BF16) for i in range(2)]
            aT = sb(p6, "aT", [128, 32, TB], BF16)
            rf = [sb(p6, f"rf{i}", [128, 2 * TB], F32) for i in range(2)]
            x1in = [sb(p6, f"x1in{i}", [128, D], F32) for i in range(2)]
            x2t = [sb(p6, f"x2t{i}", [128, D], F32) for i in range(2)]
            yt = x2t
            junk6 = sb(p6, "junk6", [128, D], BF16)
            ss6 = sb(p6, "ss6", [128, NT], F32)
            rs6 = sb(p6, "rs6", [128, NT], F32)
            pf1 = [ps(p6, f"pf1_{i}", [128, 512], F32) for i in range(3)]
            pf2 = [ps(p6, f"pf2_{i}", [128, 512], F32) for i in range(4)]
            for blk in range(T // TB):
                hb_ = blk % 2
                c.dma('sp', hin[hb_][:], h2T_s[:, :, blk * TB:(blk + 1) * TB], ['h2T_s'], [f'hin{hb_}'], f'hin{hb_}')
                for f2 in range(16):
                    pb = f2 % 3
                    for sub in range(2):
                        fc = f2 * 2 + sub
                        for k in range(8):
                            c.op('pe', lambda e: e.matmul(pf1[pb][:, sub * TB:(sub + 1) * TB], lhsT=W1[:, k, fc * 128:(fc + 1) * 128], rhs=hin[hb_][:, k, :],
                                                          start=(k == 0), stop=(k == 7)), [f'W1_{fc // 4}', f'hin{hb_}'], [f'pf1_{pb}'])
                    rb = f2 % 2
                    c.op('act', lambda e: e.activation(out=rf[rb][:], in_=pf1[pb][:, :], func=AF.Relu), [f'pf1_{pb}'], [f'rf{rb}'])
                    eng = 'dve' if f2 % 2 == 0 else 'pool'
                    c.op(eng, lambda e: e.tensor_tensor(out=aT[:, f2 * 2:f2 * 2 + 2, :].rearrange("p a t -> p (a t)"), in0=rf[rb][:], in1=rf[rb][:], op=ALU.mult),
                         [f'rf{rb}'], ['aT'])
                for ti in range(TB // 128):
                    tt = blk * (TB // 128) + ti
                    xb = tt % 2
                    c.dma('sp', x1in[xb][:], x1_s[tt * 128:(tt + 1) * 128, :], ['x1_s'], [f'x1in{xb}'], f'x1in{xb}')
                    for hf in range(2):
                        pb = (ti * 2 + hf) % 4
                        for fc in range(32):
                            c.op('pe', lambda e: e.matmul(pf2[pb][:, :], lhsT=aT[:, fc, ti * 128:(ti + 1) * 128], rhs=W2[:, fc, hf * 512:(hf + 1) * 512],
                                                          start=(fc == 0), stop=(fc == 31)), ['aT', f'W2_{fc // 4}'], [f'pf2_{pb}'])
                        c.op('dve', lambda e: e.tensor_tensor(out=x2t[xb][:, hf * 512:(hf + 1) * 512], in0=pf2[pb][:, :],
                                                              in1=x1in[xb][:, hf * 512:(hf + 1) * 512], op=ALU.add),
                             [f'pf2_{pb}', f'x1in{xb}'], [f'x2t{xb}'])
                    c.op('act', lambda e: e.activation(out=junk6[:], in_=x2t[xb][:], func=AF.Square, accum_out=ss6[:, tt:tt + 1]),
                         [f'x2t{xb}'], [f'ss6_{tt}', 'junk6'])
                    c.op('pool', lambda e: e.tensor_scalar(out=rs6[:, tt:tt + 1], in0=ss6[:, tt:tt + 1], scalar1=1.0 / D, scalar2=EPS,
                                                           op0=ALU.mult, op1=ALU.add), [f'ss6_{tt}'], [f'rs6_{tt}'])
                    c.op('pool', lambda e: e.tensor_tensor(out=rs6[:, tt:tt + 1], in0=rs6[:, tt:tt + 1], in1=mhalf_t[:], op=ALU.pow),
                         [f'rs6_{tt}', 'mhalf_t'], [f'rs6_{tt}'])
                    c.op('dve', lambda e: e.scalar_tensor_tensor(out=yt[xb][:], in0=x2t[xb][:], scalar=rs6[:, tt:tt + 1], in1=nfw_bc[:],
                                                                 op0=ALU.mult, op1=ALU.mult), [f'x2t{xb}', f'rs6_{tt}', 'nfw_bc'], [f'x2t{xb}'])
                    c.dma('sp', y_d[tt * 128:(tt + 1) * 128, :], yt[xb][:], [f'x2t{xb}'], ['y_d'], f'yt{xb}')
            c.barrier()
        w_pre.close()
        c.barrier(engines=('sp',))
    print(f"[build] instructions={c.nins} waits={c.nwait}")
    return nc


def make_in_maps(inputs):
    f = lambda a: np.ascontiguousarray(np.asarray(a, dtype=np.float32))
    cst = _consts()
    shared = {
        "norm1_w": f(inputs["norm1_w"]).reshape(1, D),
        "w_in": f(inputs["w_in"])[0],
        "conv_wt": np.ascontiguousarray(f(inputs["conv_a_w"])[0].T),
        "a_log": f(inputs["a_log"]).reshape(8, 1),
        "dt_bias": f(inputs["dt_bias"]).reshape(8, 1),
        "norm_a_w": f(inputs["norm_a_w"]).reshape(1, 128),
        "rel_bias_table": f(inputs["rel_bias_table"]),
        "w_gate": f(inputs["w_gate"])[0],
        "b_gate_t": np.ascontiguousarray(f(inputs["b_gate"]).reshape(16, 128).T),
        "w_proj_a": f(inputs["w_proj_a"])[0],
        "w_proj_b": f(inputs["w_proj_b"])[0],
        "w_out": f(inputs["w_out"])[0],
        "norm2_w": f(inputs["norm2_w"]).reshape(1, D),
        "w_ff1": f(inputs["w_ff1"])[0],
        "w_ff2": f(inputs["w_ff2"])[0],
        "norm_final_w": f(inputs["norm_final_w"]).reshape(1, D),
        "c_ident": cst['ident'],
        "c_sel": cst['sel'],
        "c_negm": cst['negm'],
        "c_oh": cst['oh'],
        "c_J": cst['J'],
        "c_visneg": cst['visneg'],
        "c_pw": cst['pw'],
    }
    x = f(inputs["x"])
    return [dict(shared, x=x[b]) for b in range(x.shape[0])]


def kernel(**inputs):
    nc = build_program(debug=False)
    in_maps = make_in_maps(inputs)
    res = run_bass_kernel_spmd(nc, in_maps, core_ids=list(range(8)))
    return np.stack([np.asarray(r["y"], dtype=np.float32) for r in res.results], axis=0)
```

```python
from contextlib import ExitStack
import math
import numpy as np
import ml_dtypes
import concourse.bass as bass
import concourse.mybir as mybir
from concourse.bass_utils import run_bass_kernel_spmd

F32 = mybir.dt.float32
BF16 = mybir.dt.bfloat16
AF = mybir.ActivationFunctionType
ALU = mybir.AluOpType
AX = mybir.AxisListType

T = 4096
D = 1024
NT = T // 128
NB = T // 512
DIN = 7768
EPS = 1e-6
C_QA, C_KA, C_VA, C_ZA, C_BA, C_AA, C_QB, C_KB, C_VB, C_IQ, C_IK, C_IW = (
    0, 1024, 2048, 3072, 4096, 4104, 4112, 5136, 6160, 7184, 7696, 7760)


class Ctx:
    def __init__(self, nc):
        self.nc = nc
        self.eng = {'pe': nc.tensor, 'act': nc.scalar, 'dve': nc.vector, 'pool': nc.gpsimd, 'sp': nc.sync}
        self.esem = {k: nc.alloc_semaphore(name='s_' + k) for k in ('pe', 'act', 'dve', 'pool')}
        self.ecnt = {k: 0 for k in self.esem}
        self.seen = {k: {} for k in self.eng}
        self.W = {}
        self.R = {}
        self.dsem = {}
        self.BK = {}
        self.bank_of = {}
        self.nwait = 0
        self.nins = 0

    def _deps(self, reads, writes):
        deps = {}

        def add(d):
            for name, (sem, val) in d.items():
                if name not in deps or deps[name][1] < val:
                    deps[name] = (sem, val)
        for k in reads:
            add(self.W.get(k, {}))
        for k in writes:
            add(self.W.get(k, {}))
            add(self.R.get(k, {}))
        return deps

    def _wait(self, e, deps):
        for name, (sem, val) in deps.items():
            if e == 'pe' and name == 'pe':
                continue
            if self.seen[e].get(name, 0) >= val:
                continue
            self.eng[e].wait_ge(sem, val)
            self.seen[e][name] = val
            self.nwait += 1

    def _commit(self, name, sem, val, reads, writes):
        for k in writes:
            self.W[k] = {name: (sem, val)}
            self.R[k] = {}
        for k in reads:
            self.R.setdefault(k, {})[name] = (sem, val)

    def op(self, e, fn, reads=(), writes=()):
        deps = self._deps(reads, writes)
        banks = {self.bank_of[k] for k in list(reads) + list(writes) if k in self.bank_of}
        for bk in banks:
            for name, (sem, val) in self.BK.get(bk, {}).items():
                if name != e and (name not in deps or deps[name][1] < val):
                    deps[name] = (sem, val)
        self._wait(e, deps)
        ins = fn(self.eng[e])
        self.ecnt[e] += 1
        ins.then_inc(self.esem[e], 1)
        self.nins += 1
        self._commit(e, self.esem[e], self.ecnt[e], reads, writes)
        for bk in banks:
            self.BK.setdefault(bk, {})[e] = (self.esem[e], self.ecnt[e])

    def dma(self, q, out, in_, reads, writes, slot, **kw):
        slot = f'{q}_{slot}'
        self._wait(q, self._deps(reads, writes))
        if slot not in self.dsem:
            self.dsem[slot] = [self.nc.alloc_semaphore(name='d_' + slot), 0]
        ds = self.dsem[slot]
        if ds[1] > 0 and self.seen[q].get('d_' + slot, 0) < ds[1]:
            self.eng[q].wait_ge(ds[0], ds[1])
            self.seen[q]['d_' + slot] = ds[1]
            self.nwait += 1
        ins = self.eng[q].dma_start(out=out, in_=in_, **kw)
        ds[1] += 16
        ins.then_inc(ds[0], 16)
        self.nins += 1
        self._commit('d_' + slot, ds[0], ds[1], reads, writes)

    def barrier(self, engines=('pe', 'act', 'dve', 'pool', 'sp')):
        allsem = {k: (self.esem[k], self.ecnt[k]) for k in self.esem if self.ecnt[k] > 0}
        for s, (sem, v) in self.dsem.items():
            if v > 0:
                allsem['d_' + s] = (sem, v)
        for e in engines:
            for name, (sem, val) in allsem.items():
                if e == name:
                    continue
                if self.seen[e].get(name, 0) >= val:
                    continue
                self.eng[e].wait_ge(sem, val)
                self.seen[e][name] = val
        self.W.clear()
        self.R.clear()
        self.BK.clear()


def _consts():
    c = {}
    c['ident'] = np.eye(128, dtype=np.float32)
    sel = np.zeros((48, 32, 128), np.float32)
    for q, base in enumerate((16, 40, 8, 0)):
        for h in range(8):
            sel[base + h, q * 8 + h, :] = 1.0
    c['sel'] = sel
    i = np.arange(128)
    same = (i[:, None] // 64) == (i[None, :] // 64)
    NEG = -30000.0
    negS = np.where(same & (i[:, None] < i[None, :]), 0.0, NEG)
    negI = np.where(same & (i[:, None] <= i[None, :]), 0.0, NEG)
    negST = np.where(same & (i[None, :] < i[:, None]), 0.0, NEG)
    rel = (511 - np.arange(1152)).astype(np.int32)
    half, max_exact = 16, 8
    base = np.where(rel > 0, half, 0)
    nn = np.abs(rel)
    n_f = np.maximum(nn, 1).astype(np.float32)
    large = max_exact + (np.log(n_f / np.float32(max_exact)) / np.float32(math.log(128 / max_exact))
                         * np.float32(half - max_exact)).astype(np.int32)
    large = np.minimum(large, half - 1)
    bucket = base + np.where(nn < max_exact, nn, large)
    ohm = np.zeros((32, 1152), np.float32)
    ohm[bucket, np.arange(1152)] = 1.0
    c['oh'] = ohm
    c['J'] = np.ascontiguousarray(np.eye(128, dtype=np.float32)[::-1])
    tl = np.arange(128)
    c['visneg'] = np.where((tl[:, None] < 64) & (tl[None, :] >= 64), -30000.0, 0.0).astype(np.float32)
    c['pw'] = np.ascontiguousarray(np.broadcast_to((2.0 ** -np.arange(1, 17)).astype(np.float32)[None, :], (128, 16)))
    c['negm'] = np.ascontiguousarray(np.stack([negS, negI, negST], axis=1).astype(np.float32))
    return c


def build_program(debug=False):
    nc = bass.Bass("TRN2", target_bir_lowering=False)
    ikind = "ExternalOutput" if debug else "Internal"

    def din(name, shape, dt=F32):
        return nc.dram_tensor(name, list(shape), dt, kind="ExternalInput").ap()

    def dscr(name, shape, dt=BF16):
        return nc.dram_tensor(name, list(shape), dt, kind=ikind).ap()

    x_d = din("x", [T, D])
    n1w_d = din("norm1_w", [1, D])
    w_in_d = din("w_in", [D, DIN])
    convw_d = din("conv_wt", [3072, 4])
    alog_d = din("a_log", [8, 1])
    dtb_d = din("dt_bias", [8, 1])
    naw_d = din("norm_a_w", [1, 128])
    rel_d = din("rel_bias_table", [32, 8])
    wg_d = din("w_gate", [D, 2048])
    bg_d = din("b_gate_t", [128, 16])
    wpa_d = din("w_proj_a", [D, D])
    wpb_d = din("w_proj_b", [D, D])
    wo_d = din("w_out", [D, D])
    n2w_d = din("norm2_w", [1, D])
    wf1_d = din("w_ff1", [D, 4096])
    wf2_d = din("w_ff2", [4096, D])
    nfw_d = din("norm_final_w", [1, D])
    ident_d = din("c_ident", [128, 128])
    sel_d = din("c_sel", [48, 32, 128])
    negm_d = din("c_negm", [128, 3, 128])
    oh_d = din("c_oh", [32, 1152])
    J_d = din("c_J", [128, 128])
    visneg_d = din("c_visneg", [128, 128])
    pw_d = din("c_pw", [128, 16])
    y_d = nc.dram_tensor("y", [T, D], F32, kind="ExternalOutput").ap()

    qaT_s = dscr("qaT_s", [1024, T])
    kaT_s = dscr("kaT_s", [1024, T])
    vaT_s = dscr("vaT_s", [1024, T])
    za_s = dscr("za_s", [T, 1024])
    qbT_s = dscr("qbT_s", [1024, T])
    kbT_s = dscr("kbT_s", [1024, T])
    vb_s = dscr("vb_s", [T, 1024])
    iqT_s = dscr("iqT_s", [512, T])
    ikT_s = dscr("ikT_s", [128, T])
    gT_s = dscr("gT_s", [2048, T])
    gate_s = dscr("gate_s", [2, 8, T], F32)
    oa_s = dscr("oa_s", [T, 1024])
    ob_s = dscr("ob_s", [T, 1024])
    fr_s = dscr("fr_s", [8, 1152], F32)
    x1_s = dscr("x1_s", [T, D], F32)
    h2T_s = dscr("h2T_s", [128, 8, T])
    dbg = {}
    if debug:
        dbg['hT'] = nc.dram_tensor("dbg_hT", [128, 8, T], BF16, kind="ExternalOutput").ap()
        dbg['iw'] = nc.dram_tensor("dbg_iw", [128, NT, 8], F32, kind="ExternalOutput").ap()

    c = Ctx(nc)
    top = ExitStack()

    def sb(es, name, shape, dt):
        return es.enter_context(nc.sbuf_tensor(name, list(shape), dt))

    def ps(es, name, shape, dt):
        return es.enter_context(nc.psum_tensor(name, list(shape), dt))

    with top:
        ident_f = sb(top, "ident_f", [128, 128], F32)
        ident_b = sb(top, "ident_b", [128, 128], BF16)
        ones_b = sb(top, "ones_b", [128, 128], BF16)
        eps_t = sb(top, "eps_t", [128, 1], F32)
        mhalf_t = sb(top, "mhalf_t", [128, 1], F32)
        iw_all = sb(top, "iw_all", [128, NT, 8], F32)
        gdn_top = ExitStack()
        SRall = sb(gdn_top, "SRall", [48, T], F32)
        tokS = sb(gdn_top, "tokS", [128, NT, 48], F32)
        eglast = sb(gdn_top, "eglast", [128, 8, 64], F32)
        c.dma('sp', ident_f[:], ident_d, [], ['ident_f'], 'c0')
        c.dma('pool', ident_b[:], ident_d, [], ['ident_b'], 'c1')
        c.op('dve', lambda e: e.memset(ones_b[:], 1.0), [], ['ones_b'])
        c.op('dve', lambda e: e.memset(eps_t[:], EPS), [], ['eps_t'])
        c.op('dve', lambda e: e.memset(mhalf_t[:], -0.5), [], ['mhalf_t'])

        with ExitStack() as p12:
            hT = sb(p12, "hT", [128, 8, T], BF16)
            with ExitStack() as p1:
                n1w_bc = sb(p1, "n1w_bc", [128, D], F32)
                c.dma('sp', n1w_bc[:], n1w_d.partition_broadcast(128), [], ['n1w_bc'], 'c2')
                xt = [sb(p1, f"xt{i}", [128, D], F32) for i in range(3)]
                hb = [sb(p1, f"hb{i}", [128, D], BF16) for i in range(2)]
                junk = sb(p1, "junk1", [128, D], BF16)
                ss = sb(p1, "ss", [128, NT], F32)
                rs = sb(p1, "rs", [128, NT], F32)
                rstd = sb(p1, "rstd", [128, NT], F32)
                pst = [ps(p1, f"pst{i}", [128, 8, 128], BF16) for i in range(2)]

                def stA(i):
                    s = i % 3
                    c.dma('sp', xt[s][:], x_d[i * 128:(i + 1) * 128, :], [], [f'xt{s}'], f'xt{s}')
                    c.op('act', lambda e: e.activation(out=junk[:], in_=xt[s][:], func=AF.Square,
                                                       accum_out=ss[:, i:i + 1]), [f'xt{s}'], [f'ss{i}', 'junk1'])

                def stB(i):
                    s = i % 3
                    c.op('pool', lambda e: e.tensor_scalar(out=rs[:, i:i + 1], in0=ss[:, i:i + 1],
                                                           scalar1=1.0 / D, scalar2=EPS, op0=ALU.mult, op1=ALU.add),
                         [f'ss{i}'], [f'rs{i}'])
                    c.op('pool', lambda e: e.tensor_tensor(out=rstd[:, i:i + 1], in0=rs[:, i:i + 1],
                                                           in1=mhalf_t[:], op=ALU.pow),
                         [f'rs{i}', 'mhalf_t'], [f'rstd{i}'])
                    c.op('dve', lambda e: e.scalar_tensor_tensor(out=hb[i % 2][:], in0=xt[s][:],
                                                                 scalar=rstd[:, i:i + 1], in1=n1w_bc[:],
                                                                 op0=ALU.mult, op1=ALU.mult),
                         [f'xt{s}', f'rstd{i}', 'n1w_bc'], [f'hb{i % 2}'])

                def stC(i):
                    b = i % 2
                    for k in range(8):
                        c.op('pe', lambda e: e.transpose(out=pst[b][:, k, :], in_=hb[b][:, k * 128:(k + 1) * 128],
                                                         identity=ident_b[:]),
                             [f'hb{b}', 'ident_b'], [f'pst{b}'])
                    c.op('act', lambda e: e.copy(out=hT[:, :, i * 128:(i + 1) * 128], in_=pst[b][:]),
                         [f'pst{b}'], [f'hT{i // 4}'])

                for i in range(NT + 2):
                    if i < NT:
                        stA(i)
                    if 0 <= i - 1 < NT:
                        stB(i - 1)
                    if 0 <= i - 2 < NT:
                        stC(i - 2)
                c.barrier()
            if debug:
                c.dma('sp', dbg['hT'], hT[:], [f'hT{b}' for b in range(NB)], [], 'dbg0')

            with ExitStack() as p2:
                wch = [sb(p2, f"wch{i}", [128, 8, 512], BF16) for i in range(2)]
                stg = [sb(p2, f"stg{i}", [128, T], BF16) for i in range(2)]
                tstg = [sb(p2, f"tstg{i}", [128, 512], BF16) for i in range(3)]
                pres = [sb(p2, f"pre{i}", [128, T + 4], F32) for i in range(2)]
                accs = [sb(p2, f"acc{i}", [128, T], F32) for i in range(2)]
                pre, acc = pres[0], accs[0]
                sq = sb(p2, "sq", [128, T], BF16)
                cw = sb(p2, "cw", [128, 24, 4], F32)
                bg = sb(p2, "bg", [128, 16], F32)
                psa = [ps(p2, f"psa{i}", [128, 512], F32) for i in range(3)]
                psn = [ps(p2, f"psn{i}", [128, 512], F32) for i in range(2)]
                c.dma('sp', cw[:], convw_d.rearrange("(c p) j -> p c j", p=128), [], ['cw'], 'c3')
                c.dma('sp', bg[:], bg_d, [], ['bg'], 'c4')
                c.op('dve', lambda e: e.memset(pres[0][:, 0:3], 0.0), [], ['pre0'])
                c.op('dve', lambda e: e.memset(pres[1][:, 0:3], 0.0), [], ['pre1'])
                state = {'w': 0, 'ps': 0, 'stg': 0, 'ts': 0, 'ev': 0}

                def load_w(src_d, col0, ncols, dst_col=0, slot=None):
                    s = state['w'] % 2 if slot is None else slot
                    if slot is None:
                        state['w'] += 1
                    c.dma('pool', wch[s][:, :, dst_col:dst_col + ncols],
                          src_d.rearrange("(k p) c -> p k c", p=128)[:, :, col0:col0 + ncols],
                          [], [f'wch{s}'], f'wch{s}')
                    return s

                def fm_block(ws, wc0, m, blk):
                    p = state['ps'] % 3
                    state['ps'] += 1
                    for k in range(8):
                        c.op('pe', lambda e: e.matmul(psa[p][0:m, :], lhsT=wch[ws][:, k, wc0:wc0 + m],
                                                      rhs=hT[:, k, blk * 512:(blk + 1) * 512],
                                                      start=(k == 0), stop=(k == 7)),
                             [f'wch{ws}', f'hT{blk}'], [f'psa{p}'])
                    return p

                def evac_copy(out_ap, in_ap, rkeys, wkeys):
                    if state['ev'] % 2 == 0:
                        c.op('act', lambda e: e.copy(out=out_ap, in_=in_ap), rkeys, wkeys)
                    else:
                        c.op('dve', lambda e: e.tensor_copy(out=out_ap, in_=in_ap), rkeys, wkeys)
                    state['ev'] += 1

                def fm_plain(src_d, col0, nchunks, dst_s, row0=0):
                    for g0 in range(0, nchunks, 4):
                        gn = min(4, nchunks - g0)
                        ws = load_w(src_d, col0 + g0 * 128, gn * 128)
                        for cc in range(gn):
                            s = state['stg'] % 2
                            state['stg'] += 1
                            for blk in range(NB):
                                p = fm_block(ws, cc * 128, 128, blk)
                                evac_copy(stg[s][:, blk * 512:(blk + 1) * 512], psa[p][:],
                                          [f'psa{p}'], [f'stg{s}'])
                            r = row0 + (g0 + cc) * 128
                            c.dma('sp', dst_s[r:r + 128, :], stg[s][:], [f'stg{s}'], [dst_s.tensor.name], f'stg{s}')

                fm_plain(w_in_d, C_QB, 8, qbT_s)
                fm_plain(w_in_d, C_KB, 8, kbT_s)
                fm_plain(w_in_d, C_IQ, 4, iqT_s)
                ws = state['w'] % 2
                state['w'] += 1
                load_w(w_in_d, C_IK, 64, dst_col=0, slot=ws)
                load_w(w_in_d, C_IK, 64, dst_col=64, slot=ws)
                s = state['stg'] % 2
                state['stg'] += 1
                for blk in range(NB):
                    p = fm_block(ws, 0, 128, blk)
                    evac_copy(stg[s][:, blk * 512:(blk + 1) * 512], psa[p][:], [f'psa{p}'], [f'stg{s}'])
                c.dma('sp', ikT_s[:, :], stg[s][:], [f'stg{s}'], ['ikT_s'], f'stg{s}')
                for g0 in range(0, 16, 4):
                    ws = load_w(wg_d, g0 * 128, 512)
                    for cc in range(4):
                        s = state['stg'] % 2
                        state['stg'] += 1
                        ch = g0 + cc
                        for blk in range(NB):
                            p = fm_block(ws, cc * 128, 128, blk)
                            c.op('act', lambda e: e.activation(out=stg[s][:, blk * 512:(blk + 1) * 512], in_=psa[p][:],
                                                               func=AF.Sigmoid, bias=bg[:, ch:ch + 1], scale=1.0),
                                 [f'psa{p}', 'bg'], [f'stg{s}'])
                        c.dma('sp', gT_s[ch * 128:(ch + 1) * 128, :], stg[s][:], [f'stg{s}'], ['gT_s'], f'stg{s}')
                ws = state['w'] % 2
                state['w'] += 1
                load_w(w_in_d, C_BA, 16, dst_col=0, slot=ws)
                load_w(w_in_d, C_IW, 8, dst_col=16, slot=ws)
                for gi2, wc0 in enumerate((0, 8)):
                    for blk in range(NB):
                        p = fm_block(ws, wc0, 8, blk)
                        evac_copy(accs[0][0:8, blk * 512:(blk + 1) * 512], psa[p][0:8, :], [f'psa{p}'], ['acc0'])
                    c.dma('sp', gate_s[gi2], accs[0][0:8, :], ['acc0'], ['gate_s'], 'acc')
                p = state['ps'] % 3
                state['ps'] += 1
                for tt in range(NT):
                    for k in range(8):
                        c.op('pe', lambda e: e.matmul(psa[p][:, tt * 8:(tt + 1) * 8], lhsT=hT[:, k, tt * 128:(tt + 1) * 128],
                                                      rhs=wch[ws][:, k, 16:24], start=(k == 0), stop=(k == 7)),
                             [f'wch{ws}', f'hT{tt // 4}'], [f'psa{p}'])
                c.op('act', lambda e: e.copy(out=iw_all[:].rearrange("p t h -> p (t h)"), in_=psa[p][:, 0:NT * 8]),
                     [f'psa{p}'], ['iw_all'])
                for (col0, dst_s, fn) in ((C_ZA, za_s, AF.Silu), (C_VB, vb_s, None)):
                    for g0 in range(2):
                        ws = load_w(w_in_d, col0 + g0 * 512, 512)
                        for tt in range(NT):
                            p = state['ps'] % 3
                            state['ps'] += 1
                            for k in range(8):
                                c.op('pe', lambda e: e.matmul(psa[p][:], lhsT=hT[:, k, tt * 128:(tt + 1) * 128],
                                                              rhs=wch[ws][:, k, :], start=(k == 0), stop=(k == 7)),
                                     [f'wch{ws}', f'hT{tt // 4}'], [f'psa{p}'])
                            s = state['ts'] % 3
                            state['ts'] += 1
                            if fn is None:
                                evac_copy(tstg[s][:], psa[p][:], [f'psa{p}'], [f'tstg{s}'])
                            else:
                                c.op('act', lambda e: e.activation(out=tstg[s][:], in_=psa[p][:], func=fn),
                                     [f'psa{p}'], [f'tstg{s}'])
                            c.dma('sp', dst_s[tt * 128:(tt + 1) * 128, g0 * 512:(g0 + 1) * 512], tstg[s][:],
                                  [f'tstg{s}'], [dst_s.tensor.name], f'tstg{s}')
                chunks = []
                for gi, (col0, dst_s) in enumerate(((C_QA, qaT_s), (C_KA, kaT_s), (C_VA, vaT_s))):
                    for g0 in range(0, 8, 4):
                        for cc in range(4):
                            chunks.append((gi, col0, dst_s, g0, cc))
                wslot = {}

                def stage_A(idx):
                    gi, col0, dst_s, g0, cc = chunks[idx]
                    if cc == 0:
                        wslot[(gi, g0)] = load_w(w_in_d, col0 + g0 * 128, 512)
                    ws = wslot[(gi, g0)]
                    ch = gi * 8 + g0 + cc
                    pb_ = ch % 2
                    pre, acc = pres[pb_], accs[pb_]
                    kp, ka = f'pre{pb_}', f'acc{pb_}'
                    for blk in range(NB):
                        p = fm_block(ws, cc * 128, 128, blk)
                        c.op('act', lambda e: e.copy(out=pre[:, 3 + blk * 512:3 + (blk + 1) * 512], in_=psa[p][:]),
                             [f'psa{p}'], [kp])
                    c.op('dve', lambda e: e.tensor_scalar(out=acc[:], in0=pre[:, 0:T], scalar1=cw[:, ch, 0:1],
                                                          scalar2=None, op0=ALU.mult), [kp, 'cw'], [ka])
                    for j in range(1, 4):
                        c.op('dve', lambda e: e.scalar_tensor_tensor(out=acc[:], in0=pre[:, j:j + T],
                                                                     scalar=cw[:, ch, j:j + 1], in1=acc[:],
                                                                     op0=ALU.mult, op1=ALU.add),
                             [kp, 'cw', ka], [ka])

                def stage_B(idx):
                    gi, col0, dst_s, g0, cc = chunks[idx]
                    ch = gi * 8 + g0 + cc
                    pb_ = ch % 2
                    pre, acc = pres[pb_], accs[pb_]
                    kp, ka = f'pre{pb_}', f'acc{pb_}'
                    s = state['stg'] % 2
                    state['stg'] += 1
                    if gi == 2:
                        c.op('act', lambda e: e.activation(out=stg[s][:], in_=acc[:], func=AF.Silu),
                             [ka], [f'stg{s}'])
                    else:
                        c.op('act', lambda e: e.activation(out=acc[:], in_=acc[:], func=AF.Silu), [ka], [ka])
                        c.op('act', lambda e: e.activation(out=sq[:], in_=acc[:], func=AF.Square), [ka], ['sq'])
                        for blk in range(NB):
                            q = blk % 2
                            c.op('pe', lambda e: e.matmul(psn[q][:], lhsT=ones_b[:], rhs=sq[:, blk * 512:(blk + 1) * 512],
                                                          start=True, stop=True), ['sq', 'ones_b'], [f'psn{q}'])
                            c.op('act', lambda e: e.activation(out=pre[:, 4 + blk * 512:4 + (blk + 1) * 512], in_=psn[q][:],
                                                               func=AF.Ln, bias=eps_t[:], scale=1.0),
                                 [f'psn{q}', 'eps_t'], [kp])
                        c.op('act', lambda e: e.activation(out=pre[:, 4:4 + T], in_=pre[:, 4:4 + T], func=AF.Exp, scale=-0.5), [kp], [kp])
                        sc = (128.0 ** -0.5) if gi == 0 else 1.0
                        c.op('dve', lambda e: e.scalar_tensor_tensor(out=stg[s][:], in0=acc[:], scalar=sc, in1=pre[:, 4:4 + T],
                                                                     op0=ALU.mult, op1=ALU.mult),
                             [ka, kp], [f'stg{s}'])
                    r = (g0 + cc) * 128
                    c.dma('sp', dst_s[r:r + 128, :], stg[s][:], [f'stg{s}'], [dst_s.tensor.name], f'stg{s}')

                stage_A(0)
                for idx in range(len(chunks)):
                    if idx + 1 < len(chunks):
                        stage_A(idx + 1)
                    stage_B(idx)
                c.barrier()
                if debug:
                    c.dma('sp', dbg['iw'], iw_all[:], ['iw_all'], [], 'dbg0')
            c.barrier()


        with ExitStack() as pro:
            ba_r = sb(pro, "ba_r", [8, T], F32)
            aa_r = sb(pro, "aa_r", [8, T], F32)
            gc_r = sb(pro, "gc_r", [8, T], F32)
            tA = sb(pro, "tA", [8, T], F32)
            tB = sb(pro, "tB", [8, T], F32)
            rmask = sb(pro, "rmask", [8, T], F32)
            alog = sb(pro, "alog", [8, 1], F32)
            dtb = sb(pro, "dtb", [8, 1], F32)
            nA = sb(pro, "nA", [8, 1], F32)
            c.dma('sp', ba_r[:], gate_s[0], ['gate_s'], ['ba_r'], 'c0')
            c.dma('sp', aa_r[:], gate_s[1], ['gate_s'], ['aa_r'], 'c1')
            c.dma('sp', alog[:], alog_d, [], ['alog'], 'c2')
            c.dma('sp', dtb[:], dtb_d, [], ['dtb'], 'c3')
            c.op('act', lambda e: e.activation(out=ba_r[:], in_=ba_r[:], func=AF.Sigmoid), ['ba_r'], ['ba_r'])
            c.op('act', lambda e: e.activation(out=aa_r[:], in_=aa_r[:], func=AF.Exp, bias=dtb[:, 0:1], scale=1.0),
                 ['aa_r', 'dtb'], ['aa_r'])
            c.op('act', lambda e: e.activation(out=aa_r[:], in_=aa_r[:], func=AF.Ln, bias=1.0, scale=1.0), ['aa_r'], ['aa_r'])
            c.op('act', lambda e: e.activation(out=nA[:], in_=alog[:], func=AF.Exp), ['alog'], ['nA'])
            c.op('dve', lambda e: e.tensor_scalar(out=nA[:], in0=nA[:], scalar1=-1.0, scalar2=None, op0=ALU.mult), ['nA'], ['nA'])
            c.op('dve', lambda e: e.tensor_scalar(out=aa_r[:], in0=aa_r[:], scalar1=nA[:, 0:1], scalar2=None, op0=ALU.mult),
                 ['aa_r', 'nA'], ['aa_r'])
            c.op('dve', lambda e: e.memset(rmask[:], 1.0), [], ['rmask'])
            c.op('dve', lambda e: e.memset(rmask[:, 0:T:64], 0.0), ['rmask'], ['rmask'])
            c.op('dve', lambda e: e.tensor_tensor_scan(out=gc_r[:], data0=rmask[:], data1=aa_r[:], initial=0.0,
                                                       op0=ALU.mult, op1=ALU.add), ['rmask', 'aa_r'], ['gc_r'])
            c.dma('sp', SRall[8:16, :], gc_r[:], ['gc_r'], ['SRall'], 'c0')
            c.op('dve', lambda e: e.tensor_scalar(out=tA[:], in0=gc_r[:], scalar1=-1.0, scalar2=None, op0=ALU.mult), ['gc_r'], ['tA'])
            c.dma('sp', SRall[0:8, :], tA[:], ['tA'], ['SRall'], 'c1')
            c.dma('sp', SRall[16:24, :], ba_r[:], ['ba_r'], ['SRall'], 'c2')
            c.op('act', lambda e: e.activation(out=tB[:], in_=gc_r[:], func=AF.Exp), ['gc_r'], ['tB'])
            c.dma('sp', SRall[40:48, :], tB[:], ['tB'], ['SRall'], 'c3')
            c.op('dve', lambda e: e.tensor_tensor(out=tA[:], in0=ba_r[:], in1=tB[:], op=ALU.mult), ['ba_r', 'tB', 'tA'], ['tA'])
            c.dma('sp', SRall[24:32, :], tA[:], ['tA'], ['SRall'], 'c1')
            gc3 = gc_r[:].rearrange("p (n c) -> p n c", c=64)
            tB3 = tB[:].rearrange("p (n c) -> p n c", c=64)
            c.op('dve', lambda e: e.tensor_tensor(out=tB3, in0=gc3[:, :, 63:64].to_broadcast([8, 64, 64]), in1=gc3,
                                                  op=ALU.subtract), ['gc_r', 'tB'], ['tB'])
            c.op('act', lambda e: e.activation(out=tB[:], in_=tB[:], func=AF.Exp), ['tB'], ['tB'])
            c.dma('sp', SRall[32:40, :], tB[:], ['tB'], ['SRall'], 'c3')
            c.barrier()
        with ExitStack() as p3:
            sel = sb(p3, "sel", [48, 32, 128], F32)
            negm = sb(p3, "negm", [128, 3, 128], BF16)
            naw_bc = sb(p3, "naw_bc", [128, 128], F32)
            c.dma('sp', sel[:], sel_d, [], ['sel'], 'c0')
            c.dma('pool', negm[:], negm_d, [], ['negm'], 'c1')
            c.dma('sp', naw_bc[:], naw_d.partition_broadcast(128), [], ['naw_bc'], 'c2')
            sets = [tuple(ps(p3, f"{nm}{si}", [128, 512], F32) for nm in ("B0_", "PG_", "PM_", "PX_")) for si in range(2)]
            B0, PG, PM, PX = sets[0]
            BTs = [sets[si][3][:, 384:512].bitcast(BF16) for si in range(2)]
            allbanks = [f"{nm}{si}" for si in range(2) for nm in ("B0_", "PG_", "PM_", "PX_")]
            hbank = [sets[si][j] for si in range(2) for j in range(4)]
            c.bank_of['B0'] = 'B0_0'
            c.bank_of['PG'] = 'PG_0'
            for si in range(2):
                for k_, b_ in (('B0a', 'B0_'), ('B0b', 'B0_'), ('B0c', 'B0_'), ('PGx', 'PG_'), ('PMx', 'PM_'),
                               ('PXa', 'PX_'), ('PXb', 'PX_'), ('PXc', 'PX_'), ('BTa', 'PX_'), ('BTb', 'PX_')):
                    c.bank_of[f'{k_}{si}'] = f'{b_}{si}'
            for h_ in range(8):
                for k_ in ('PAa', 'PAb', 'PS'):
                    c.bank_of[f'{k_}{h_}'] = allbanks[h_]
            for g in range(4):
                for j in range(8):
                    tt = g * 8 + j
                    c.op('pe', lambda e: e.transpose(out=B0[:, j * 48:(j + 1) * 48], in_=SRall[:, tt * 128:(tt + 1) * 128],
                                                     identity=ident_f[0:48, 0:48]), ['SRall', 'ident_f'], ['B0'])
                c.op('act', lambda e: e.copy(out=tokS[:, g * 8:(g + 1) * 8, :].rearrange("p t r -> p (t r)"), in_=B0[:, 0:384]),
                     ['B0'], ['tokS'])
            for h in range(8):
                c.op('pe', lambda e: e.matmul(PG[:, h * 64:(h + 1) * 64], lhsT=sel[:, 8 + h, :], rhs=SRall[:, 63:T:64],
                                              start=True, stop=True), ['sel', 'SRall'], ['PG'])
            c.op('act', lambda e: e.copy(out=eglast[:].rearrange("p h n -> p (h n)"), in_=PG[:]), ['PG'], ['eglast'])
            c.barrier()
            qin = [sb(p3, f"qin{i}", [128, 512], BF16) for i in range(2)]
            kin = [sb(p3, f"kin{i}", [128, 512], BF16) for i in range(2)]
            vin = [sb(p3, f"vin{i}", [128, 512], BF16) for i in range(2)]
            kbT = [sb(p3, f"kbT{i}", [128, 128], BF16) for i in range(2)]
            kbg = [sb(p3, f"kbg{i}", [128, 128], BF16) for i in range(2)]
            vbt = [sb(p3, f"vbt{i}", [128, 128], BF16) for i in range(2)]
            EM = [sb(p3, f"EM{i}", [128, 384], F32) for i in range(2)]
            NN = [sb(p3, f"NN{i}", [128, 256], BF16) for i in range(2)]
            PP = [sb(p3, f"PP{i}", [128, 256], BF16) for i in range(2)]
            Xs = [sb(p3, f"Xs{i}", [128, 128], BF16) for i in range(2)]
            qdT_sg = sb(p3, "qdT_sg", [128, 8, 512], BF16)
            wT_sg = sb(p3, "wT_sg", [128, 8, 512], BF16)
            QK_sg = sb(p3, "QK_sg", [128, 8, 4, 128], BF16)
            kt_sg = sb(p3, "kt_sg", [128, 8, 4, 128], BF16)
            u_sg = sb(p3, "u_sg", [128, 8, 4, 128], F32)
            o_sg = sb(p3, "o_sg", [128, 8, 4, 128], F32)
            S32 = sb(p3, "S32", [128, 8, 128], F32)
            Sb = sb(p3, "Sb", [128, 8, 128], BF16)
            vnew = sb(p3, "vnew", [128, 8, 128], BF16)
            zs = [sb(p3, f"zs{i}", [128, 1024], BF16) for i in range(2)]
            oat = [sb(p3, f"oat{i}", [128, 1024], BF16) for i in range(2)]
            t1 = sb(p3, "t1", [128, 8, 128], F32)
            ssq = sb(p3, "ssq", [128, 8], F32)
            rstd8 = sb(p3, "rstd8", [128, 8], F32)
            junk3 = sb(p3, "junk3", [128, 128], BF16)
            c.op('dve', lambda e: e.memset(S32[:], 0.0), [], [f'S32_{h}' for h in range(8)])
            c.op('dve', lambda e: e.memset(Sb[:], 0.0), [], [f'Sb_{h}' for h in range(8)])
            PPs = [[sb(p3, f"PPs{si}_{i}", [128, 256], BF16) for i in range(2)] for si in range(2)]

            def gen3a(seg, h, lt, si):
                B0, PG, PM, PX = sets[si]
                BT = BTs[si]
                b = si
                sl = (seg * 8 + h) % 2
                csl = slice(seg * 512, (seg + 1) * 512)
                if lt == 0:
                    c.dma('sp', qin[sl][:], qaT_s[h * 128:(h + 1) * 128, csl], ['qaT_s'], [f'qin{sl}'], f'qin{sl}')
                    c.dma('sp', kin[sl][:], kaT_s[h * 128:(h + 1) * 128, csl], ['kaT_s'], [f'kin{sl}'], f'kin{sl}')
                    c.dma('sp', vin[sl][:], vaT_s[h * 128:(h + 1) * 128, csl], ['vaT_s'], [f'vin{sl}'], f'vin{sl}')
                tt = seg * 4 + lt
                cs = slice(lt * 128, (lt + 1) * 128)
                gs = slice(tt * 128, (tt + 1) * 128)
                kB0a, kB0b, kB0c, kPG, kPM = f'B0a{si}', f'B0b{si}', f'B0c{si}', f'PGx{si}', f'PMx{si}'
                kPXa, kPXb, kPXc, kBTa, kBTb = f'PXa{si}', f'PXb{si}', f'PXc{si}', f'BTa{si}', f'BTb{si}'
                c.op('pe', lambda e: e.matmul(B0[:, 0:128], lhsT=sel[:, 0 + h, :], rhs=SRall[:, gs], start=True, stop=True),
                     ['sel', 'SRall'], [kB0a])
                c.op('pe', lambda e: e.matmul(B0[:, 128:256], lhsT=sel[:, 8 + h, :], rhs=SRall[:, gs], start=True, stop=True),
                     ['sel', 'SRall'], [kB0b])
                c.op('pe', lambda e: e.transpose(out=BT[:, 0:128], in_=kin[sl][:, cs], identity=ident_b[:]),
                     [f'kin{sl}', 'ident_b'], [kBTa])
                c.op('pe', lambda e: e.transpose(out=BT[:, 128:256], in_=vin[sl][:, cs], identity=ident_b[:]),
                     [f'vin{sl}', 'ident_b'], [kBTb])
                for (col, sj, mj) in ((0, 16 + h, 1), (128, 16 + h, 0), (256, 24 + h, 2)):
                    c.op('pe', lambda e: e.matmul(PM[:, col:col + 128], lhsT=sel[:, sj, :], rhs=SRall[:, gs], start=True, stop=False),
                         ['sel', 'SRall'], [kPM])
                    c.op('pe', lambda e: e.matmul(PM[:, col:col + 128], lhsT=ident_b[:], rhs=negm[:, mj, :], start=False, stop=True),
                         ['ident_b', 'negm'], [kPM])
                yield
                c.op('dve', lambda e: e.tensor_tensor(out=kbT[b][:], in0=kin[sl][:, cs], in1=B0[:, 0:128], op=ALU.mult),
                     [f'kin{sl}', kB0a], [f'kbT{b}'])
                c.op('dve', lambda e: e.tensor_tensor(out=qdT_sg[:, h, cs], in0=qin[sl][:, cs], in1=B0[:, 128:256], op=ALU.mult),
                     [f'qin{sl}', kB0b], [f'qd{h}'])
                c.op('act', lambda e: e.activation(out=EM[b][:, 0:256], in_=PM[:, 0:256], func=AF.Exp,
                                                   bias=tokS[:, tt, 0 + h:1 + h], scale=1.0), [kPM, 'tokS'], [f'EMa{b}'])
                c.op('act', lambda e: e.activation(out=EM[b][:, 256:384], in_=PM[:, 256:384], func=AF.Exp,
                                                   bias=tokS[:, tt, 8 + h:9 + h], scale=1.0), [kPM, 'tokS'], [f'EMb{b}'])
                c.op('dve', lambda e: e.tensor_scalar(out=kbg[b][:], in0=BT[:, 0:128], scalar1=tokS[:, tt, 24 + h:25 + h],
                                                      scalar2=None, op0=ALU.mult), [kBTa, 'tokS'], [f'kbg{b}'])
                c.op('dve', lambda e: e.tensor_scalar(out=kt_sg[:, h, lt, :], in0=BT[:, 0:128], scalar1=tokS[:, tt, 32 + h:33 + h],
                                                      scalar2=None, op0=ALU.mult), [kBTa, 'tokS'], [f'kt{h}'])
                c.op('act', lambda e: e.activation(out=vbt[b][:], in_=BT[:, 128:256], func=AF.Copy,
                                                   scale=tokS[:, tt, 16 + h:17 + h]), [kBTb, 'tokS'], [f'vbt{b}'])
                yield
                c.op('pe', lambda e: e.matmul(PG[:, 0:128], lhsT=kin[sl][:, cs], rhs=qin[sl][:, cs], start=True, stop=True),
                     [f'kin{sl}', f'qin{sl}'], [kPG])
                c.op('pe', lambda e: e.matmul(PG[:, 128:256], lhsT=kin[sl][:, cs], rhs=kbT[b][:], start=True, stop=True),
                     [f'kin{sl}', f'kbT{b}'], [kPG])
                c.op('pe', lambda e: e.matmul(PG[:, 256:384], lhsT=kbT[b][:], rhs=kin[sl][:, cs], start=True, stop=True),
                     [f'kin{sl}', f'kbT{b}'], [kPG])
                yield
                c.op('dve', lambda e: e.tensor_tensor(out=NN[b][:], in0=PG[:, 128:384], in1=EM[b][:, 128:384], op=ALU.mult),
                     [kPG, f'EMa{b}', f'EMb{b}'], [f'NN{b}'])
                c.op('dve', lambda e: e.tensor_tensor(out=QK_sg[:, h, lt, :], in0=PG[:, 0:128], in1=EM[b][:, 0:128], op=ALU.mult),
                     [kPG, f'EMa{b}'], [f'QK{h}'])
                X = Xs[b]
                c.op('dve', lambda e: e.tensor_tensor(out=X[:], in0=ident_b[:], in1=NN[b][:, 0:128], op=ALU.subtract),
                     ['ident_b', f'NN{b}'], [f'X{b}'])
                yield
                Pk, Ptk, pkey = NN[b][:, 0:128], NN[b][:, 128:256], f'NN{b}'

                def sq(k, Pk, Ptk, pkey):
                    c.op('pe', lambda e: e.matmul(B0[:, 256:384], lhsT=Pk, rhs=Ptk, start=True, stop=True), [pkey], [kB0c])
                    if k < 5:
                        c.op('pe', lambda e: e.matmul(B0[:, 384:512], lhsT=Ptk, rhs=Pk, start=True, stop=True), [pkey], [kB0c])
                sq(1, Pk, Ptk, pkey)
                yield
                for k in range(1, 6):
                    pp = PPs[si][k % 2]
                    wd = 256 if k < 5 else 128
                    c.op('act', lambda e: e.copy(out=pp[:, 0:wd], in_=B0[:, 256:256 + wd]), [kB0c], [f'PP{si}_{k % 2}'])
                    Ptk, Pk, pkey = pp[:, 0:128], pp[:, 128:256], f'PP{si}_{k % 2}'
                    yield
                    if k < 5:
                        sq(k + 1, Pk, Ptk, pkey)
                    c.op('pe', lambda e: e.matmul(PX[:, 0:128], lhsT=Ptk, rhs=X[:], start=True, stop=True), [pkey, f'X{b}'], [kPXa])
                    yield
                    c.op('dve', lambda e: e.tensor_tensor(out=X[:], in0=X[:], in1=PX[:, 0:128], op=ALU.add),
                         [f'X{b}', kPXa], [f'X{b}'])
                yield
                c.op('pe', lambda e: e.matmul(PX[:, 128:256], lhsT=X[:], rhs=vbt[b][:], start=True, stop=True),
                     [f'X{b}', f'vbt{b}'], [kPXb])
                c.op('pe', lambda e: e.matmul(PX[:, 256:384], lhsT=kbg[b][:], rhs=X[:], start=True, stop=True),
                     [f'X{b}', f'kbg{b}'], [kPXc])
                yield
                c.op('act', lambda e: e.copy(out=u_sg[:, h, lt, :], in_=PX[:, 128:256]), [kPXb], [f'u{h}'])
                c.op('dve', lambda e: e.tensor_copy(out=wT_sg[:, h, cs], in_=PX[:, 256:384]), [kPXc], [f'wT{h}'])
                yield

            def run_pipelined(jobs, width=2):
                live = {}
                jobs = list(jobs)
                nxt = 0
                while nxt < len(jobs) or live:
                    for si in range(width):
                        if si not in live and nxt < len(jobs):
                            live[si] = jobs[nxt](si)
                            nxt += 1
                    for si in list(live):
                        try:
                            next(live[si])
                        except StopIteration:
                            del live[si]

            for seg in range(8):
                run_pipelined([(lambda si, seg=seg, h=h, lt=lt: gen3a(seg, h, lt, si)) for h in range(8) for lt in range(4)])
                for hg in range(1):
                    heads = range(8)
                    for nl in range(8):
                        n = seg * 8 + nl
                        lt, hf = nl // 2, nl % 2
                        rows = slice(64 * hf, 64 * hf + 64)
                        cols = slice(lt * 128 + 64 * hf, lt * 128 + 64 * hf + 64)
                        for h in heads:
                            pa = hbank[h]
                            c.op('pe', lambda e: e.matmul(pa[rows, 0:128], lhsT=wT_sg[:, h, cols], rhs=Sb[:, h, :], start=True, stop=True),
                                 [f'wT{h}', f'Sb_{h}'], [f'PAa{h}'])
                        for h in heads:
                            pa = hbank[h]
                            c.op('dve', lambda e: e.tensor_tensor(out=vnew[rows, h, :], in0=u_sg[rows, h, lt, :], in1=pa[rows, 0:128],
                                                                  op=ALU.subtract), [f'u{h}', f'PAa{h}'], [f'vn{h}'])
                        for h in heads:
                            pa = hbank[h]
                            c.op('pe', lambda e: e.matmul(pa[rows, 128:256], lhsT=qdT_sg[:, h, cols], rhs=Sb[:, h, :], start=True, stop=False),
                                 [f'qd{h}', f'Sb_{h}'], [f'PAb{h}'])
                            c.op('pe', lambda e: e.matmul(pa[rows, 128:256], lhsT=QK_sg[rows, h, lt, 64 * hf:64 * hf + 64], rhs=vnew[rows, h, :],
                                                          start=False, stop=True), [f'QK{h}', f'vn{h}'], [f'PAb{h}'])
                            c.op('pe', lambda e: e.matmul(pa[:, 256:384], lhsT=kt_sg[rows, h, lt, :], rhs=vnew[rows, h, :],
                                                          start=True, stop=True), [f'kt{h}', f'vn{h}'], [f'PS{h}'])
                        for h in heads:
                            pa = hbank[h]
                            c.op('dve', lambda e: e.scalar_tensor_tensor(out=S32[:, h, :], in0=S32[:, h, :], scalar=eglast[:, h, n:n + 1],
                                                                         in1=pa[:, 256:384], op0=ALU.mult, op1=ALU.add),
                                 [f'S32_{h}', 'eglast', f'PS{h}'], [f'S32_{h}'])
                            c.op('act', lambda e: e.copy(out=o_sg[rows, h, lt, :], in_=pa[rows, 128:256]), [f'PAb{h}'], [f'o{h}'])
                            c.op('act', lambda e: e.copy(out=Sb[:, h, :], in_=S32[:, h, :]), [f'S32_{h}'], [f'Sb_{h}'])
                for lt in range(4):
                    tt = seg * 4 + lt
                    zb = tt % 2
                    c.dma('sp', zs[zb][:], za_s[tt * 128:(tt + 1) * 128, :], ['za_s'], [f'zs{zb}'], f'zs{zb}')
                    for h in range(8):
                        c.op('act', lambda e: e.activation(out=junk3[:], in_=o_sg[:, h, lt, :], func=AF.Square, accum_out=ssq[:, h:h + 1]),
                             [f'o{h}'], [f'ssq{h}', 'junk3'])
                    allssq = [f'ssq{h}' for h in range(8)]
                    c.op('pool', lambda e: e.tensor_scalar(out=rstd8[:], in0=ssq[:], scalar1=1.0 / 128, scalar2=EPS, op0=ALU.mult, op1=ALU.add),
                         allssq, ['rstd8'])
                    c.op('pool', lambda e: e.tensor_tensor(out=rstd8[:], in0=rstd8[:], in1=mhalf_t[:].to_broadcast([128, 8]), op=ALU.pow),
                         ['rstd8', 'mhalf_t'], ['rstd8'])
                    c.op('dve', lambda e: e.tensor_tensor(out=t1[:], in0=o_sg[:, :, lt, :], in1=rstd8[:].unsqueeze(2).to_broadcast([128, 8, 128]),
                                                          op=ALU.mult), [f'o{h}' for h in range(8)] + ['rstd8'], ['t1'])
                    c.op('dve', lambda e: e.tensor_tensor(out=t1[:], in0=t1[:], in1=naw_bc[:].unsqueeze(1).to_broadcast([128, 8, 128]),
                                                          op=ALU.mult), ['t1', 'naw_bc'], ['t1'])
                    c.op('dve', lambda e: e.tensor_tensor(out=oat[zb][:], in0=t1[:].rearrange("p h e -> p (h e)"), in1=zs[zb][:], op=ALU.mult),
                         ['t1', f'zs{zb}'], [f'oat{zb}'])
                    c.dma('sp', oa_s[tt * 128:(tt + 1) * 128, :], oat[zb][:], [f'oat{zb}'], ['oa_s'], f'oat{zb}')
            c.barrier()


        gdn_top.close()
        NIT = 14
        SCALE_B = 128.0 ** -0.5
        with ExitStack() as p4:
            relt = sb(p4, "relt", [32, 8], F32)
            Jm = sb(p4, "Jm", [128, 128], F32)
            visneg_b = sb(p4, "visneg_b", [128, 128], BF16)
            cb = sb(p4, "cb", [128, 8], F32)
            G = sb(p4, "G", [128, 8, 1024], BF16)
            ikT = sb(p4, "ikT", [128, T], BF16)
            scores = sb(p4, "scores", [128, T], F32)
            Hk = scores[:, 0:1024]
            fr_sb = scores[0:8, 1024:2176]
            oh = scores[0:32, 2176:3328]
            mk = sb(p4, "mk", [128, T], BF16)
            maskT = sb(p4, "maskT", [128, 32, 512], BF16)
            iqb = [sb(p4, f"iqb{i}", [128, 4, 512], BF16) for i in range(2)]
            KTh = [sb(p4, f"KTh{i}", [128, T], BF16) for i in range(2)]
            Vh = [sb(p4, f"Vh{i}", [128, 32, 130], BF16) for i in range(2)]
            QTh = [sb(p4, f"QTh{i}", [128, 512], BF16) for i in range(2)]
            PTe = [sb(p4, f"PTe{i}", [128, 512], BF16) for i in range(2)]
            PTm = [sb(p4, f"PTm{i}", [128, 512], BF16) for i in range(2)]
            ME = [sb(p4, f"ME{i}", [128, 512], BF16) for i in range(10)]
            obuf = sb(p4, "obuf", [128, 4, 1024], BF16)
            sm = sb(p4, "sm", [128, 8], F32)
            wfs = sb(p4, "wfs", [128, 16], F32)
            pw_bc = sb(p4, "pw_bc", [128, 16], F32)
            mone_t = sb(p4, "mone_t", [128, 1], F32)
            pos = [sb(p4, f"pos{i}", [128, 260], F32) for i in range(2)]
            rden2 = sb(p4, "rden2", [128, 2, 2], F32)
            rden = sb(p4, "rden", [128, 4], F32)
            PSC = [ps(p4, f"PSC{i}", [128, 512], F32) for i in range(2)]
            PL = [ps(p4, f"PL{i}", [128, 512], F32) for i in range(2)]
            PO = [ps(p4, f"PO{i}", [128, 512], F32) for i in range(2)]
            BTm = ps(p4, "BTm", [128, 1024], BF16)
            PF = ps(p4, "PF", [128, 512], F32)
            c.dma('sp', relt[:], rel_d, [], ['relt'], 'c0')
            c.dma('sp', oh, oh_d, [], ['oh'], 'c1')
            c.dma('sp', Jm[:], J_d, [], ['Jm'], 'c2')
            c.dma('pool', visneg_b[:], visneg_d, [], ['visneg_b'], 'c3')
            c.dma('sp', pw_bc[:], pw_d, [], ['pw_bc'], 'c5')
            c.op('dve', lambda e: e.memset(mone_t[:], -1.0), [], ['mone_t'])
            c.dma('sp', cb[:], rel_d[15:16, :].partition_broadcast(128), [], ['cb'], 'c4')
            c.dma('sp', ikT[:], ikT_s, ['ikT_s'], ['ikT'], 'c0')
            for i in range(2):
                c.op('dve', lambda e: e.memset(Vh[i][:, :, 128:130], 1.0), [], [f'Vh{i}'])
            for j in range(3):
                c.op('pe', lambda e: e.matmul(PF[0:8, 0:384], lhsT=relt[:], rhs=oh[:, j * 384:(j + 1) * 384], start=True, stop=True),
                     ['relt', 'oh'], ['PF'])
                c.op('act', lambda e: e.activation(out=fr_sb[:, j * 384:(j + 1) * 384], in_=PF[0:8, 0:384], func=AF.Exp), ['PF'], ['fr_sb'])
            c.dma('sp', fr_s, fr_sb, ['fr_sb'], ['fr_s'], 'c1')
            for h in range(8):
                hank = bass.AP(tensor=fr_s.tensor, offset=h * 1152, ap=[[1, 128], [1, 1024]])
                c.dma('sp', Hk, hank, ['fr_s'], ['Hk'], 'c2')
                for half in range(2):
                    c.op('pe', lambda e: e.matmul(PF[:, :], lhsT=Jm[:], rhs=Hk[:, half * 512:(half + 1) * 512], start=True, stop=True),
                         ['Jm', 'Hk'], ['PF'])
                    c.op('act', lambda e: e.copy(out=G[:, h, half * 512:(half + 1) * 512], in_=PF[:, :]), ['PF'], ['G'])
            c.barrier()
            dgs = [sb(p4, f"dgs{i}", [128, 8, 128], BF16) for i in range(2)]
            rhb = [sb(p4, f"rhb{i}", [128, 512], BF16) for i in range(4)]
            maskT2 = [maskT, sb(p4, "maskT_b", [128, 32, 512], BF16)]

            scores2 = [scores, sb(p4, "scores_b", [128, T], F32)]

            def scoring(qs, qi):
                ib = qs % 2
                qt = 4 * qs + qi
                n = 128 * (qt + 1)
                sc_ = scores2[qt % 2]
                skey = f'scores{qt % 2}'
                dg = dgs[qt % 2]
                for h in range(8):
                    c.op('act', lambda e: e.activation(out=dg[:, h, :], in_=ident_b[:], func=AF.Copy, scale=iw_all[:, qt, h:h + 1]),
                         ['ident_b', 'iw_all'], [f'dgs{qt % 2}'])
                cnt_r = [0]
                for kb in range((n + 511) // 512):
                    w = min(512, n - kb * 512)

                    def smm(h):
                        j, half = h // 2, h % 2
                        rows = slice(64 * half, 64 * half + 64)
                        p = h % 2
                        c.op('pe', lambda e: e.matmul(PSC[p][:, 0:w], lhsT=iqb[ib][rows, j, qi * 128:(qi + 1) * 128],
                                                      rhs=ikT[rows, kb * 512:kb * 512 + w], start=True, stop=True),
                             [f'iqb{ib}', 'ikT'], [f'PSC{p}'])
                        r = cnt_r[0] % 4
                        cnt_r[0] += 1
                        c.op('act', lambda e: e.activation(out=rhb[r][:, 0:w], in_=PSC[p][:, 0:w], func=AF.Relu), [f'PSC{p}'], [f'rhb{r}'])
                        return r

                    lastkb = (kb == (n + 511) // 512 - 1)

                    def amm(h, r):
                        c.op('pe', lambda e: e.matmul(PF[:, 0:w], lhsT=dg[:, h, :], rhs=rhb[r][:, 0:w], start=(h == 0), stop=(h == 7 and not lastkb)),
                             [f'dgs{qt % 2}', f'rhb{r}'], ['PF'])
                        if h == 7 and lastkb:
                            c.op('pe', lambda e: e.matmul(PF[:, w - 128:w], lhsT=ident_b[:], rhs=visneg_b[:], start=False, stop=True),
                                 ['ident_b', 'visneg_b'], ['PF'])
                    rr = [None] * 8
                    rr[0] = smm(0)
                    rr[1] = smm(1)
                    for h in range(8):
                        if h + 2 < 8:
                            rr[h + 2] = smm(h + 2)
                        amm(h, rr[h])
                        yield 1.1
                    c.op('act', lambda e: e.copy(out=sc_[:, kb * 512:kb * 512 + w], in_=PF[:, 0:w]), ['PF'], [skey])

            def select(qs, qi):
                qt = 4 * qs + qi
                n = 128 * (qt + 1)
                mT_ = maskT2[qs % 2]
                mkey = f'maskT{qs % 2}'
                sc_ = scores2[qt % 2]
                skey = f'scores{qt % 2}'
                if qt >= 2:
                    c.op('dve', lambda e: e.tensor_reduce(out=sm[:, 0:1], in_=sc_[:, 0:n], op=ALU.max, axis=AX.X), [skey], ['hi'])
                    c.op('dve', lambda e: e.tensor_reduce(out=sm[:, 1:2], in_=sc_[:, 0:n - 128], op=ALU.min, axis=AX.X), [skey], ['lo'])
                    c.op('dve', lambda e: e.tensor_tensor(out=sm[:, 2:3], in0=sm[:, 0:1], in1=sm[:, 1:2], op=ALU.subtract), ['hi', 'lo'], ['w0'])
                    c.op('dve', lambda e: e.tensor_scalar(out=wfs[:, 0:NIT], in0=pw_bc[:, 0:NIT], scalar1=sm[:, 2:3], scalar2=None, op0=ALU.mult),
                         ['w0', 'pw_bc'], ['wfs'])
                    c.op('dve', lambda e: e.tensor_tensor(out=sm[:, 3:4], in0=sm[:, 1:2], in1=wfs[:, 0:1], op=ALU.add), ['lo', 'wfs'], ['mid'])
                    yield 2.0 * n / 1000.0 + 2.0
                    for it in range(1, NIT + 1):
                        c.op('dve', lambda e: e.tensor_scalar(out=mk[:, 0:n], in0=sc_[:, 0:n], scalar1=sm[:, 3:4], scalar2=None,
                                                              op0=ALU.is_ge, op1=ALU.add, accum_out=sm[:, 4:5]),
                             [skey, 'mid'], ['mk', 'cnt'])
                        last = (it == NIT)
                        c.op('dve', lambda e: e.tensor_scalar(out=sm[:, 5:6], in0=sm[:, 4:5], scalar1=255.5, scalar2=(1.0 if last else 0.5),
                                                              op0=ALU.is_ge, op1=ALU.subtract), ['cnt'], ['ge'])
                        dst, dkey = (sm[:, 1:2], 'lo') if last else (sm[:, 3:4], 'mid')
                        c.op('dve', lambda e: e.scalar_tensor_tensor(out=dst, in0=sm[:, 5:6], scalar=wfs[:, it - 1:it], in1=sm[:, 3:4],
                                                                     op0=ALU.mult, op1=ALU.add), ['ge', 'wfs', 'mid'], [dkey])
                        yield n / 1000.0 + 1.0
                    c.op('dve', lambda e: e.tensor_scalar(out=mk[:, 0:n], in0=sc_[:, 0:n], scalar1=sm[:, 1:2], scalar2=None,
                                                          op0=ALU.is_ge), [skey, 'lo'], ['mk'])
                else:
                    c.op('dve', lambda e: e.tensor_scalar(out=mk[:, 0:n], in0=sc_[:, 0:n], scalar1=-1e4, scalar2=None,
                                                          op0=ALU.is_ge), [skey], ['mk'])
                for k0 in range(0, qt + 1, 4):
                    kn = min(4, qt + 1 - k0)
                    for a in range(kn):
                        c.op('pe', lambda e: e.transpose(out=BTm[:, a * 128:(a + 1) * 128], in_=mk[:, (k0 + a) * 128:(k0 + a + 1) * 128],
                                                         identity=ident_b[:]), ['mk', 'ident_b'], ['BTm'])
                    c.op('act', lambda e: e.copy(out=mT_[:, k0:k0 + kn, qi * 128:(qi + 1) * 128],
                                                 in_=BTm[:, 0:kn * 128].rearrange("p (k t) -> p k t", t=128)), ['BTm'], [mkey])
                    yield 1.0

            def gen_attn(qs):
                mT_ = maskT2[qs % 2]
                mkey = f'maskT{qs % 2}'
                nk = 4 * qs + 4
                for h in range(8):
                    hb = (qs * 8 + h) % 2
                    c.dma('sp', KTh[hb][:, 0:nk * 128], kbT_s[h * 128:(h + 1) * 128, 0:nk * 128], ['kbT_s'], [f'KTh{hb}'], f'KTh{hb}')
                    c.dma('sp', Vh[hb][:, 0:nk, 0:128], vb_s.rearrange("(t p) c -> p t c", p=128)[:, 0:nk, h * 128:(h + 1) * 128],
                          ['vb_s'], [f'Vh{hb}'], f'Vh{hb}')
                    c.dma('sp', QTh[hb][:], qbT_s[h * 128:(h + 1) * 128, qs * 512:(qs + 1) * 512], ['qbT_s'], [f'QTh{hb}'], f'QTh{hb}')
                    started = [False, False]
                    mset = ((qs * 8 + h) % 2) * 5
                    for kt_n in range(max(0, 4 * qs - 1), nk):
                        jn = kt_n - (4 * qs - 1)
                        c.op('dve', lambda e: e.tensor_tensor(out=ME[mset + jn][:], in0=mT_[:, kt_n, :],
                                                              in1=G[:, h, 512 - 128 * jn:1024 - 128 * jn], op=ALU.mult),
                             [mkey, 'G'], [f'ME{mset + jn}'])

                    def logits(kt):
                        p = kt % 2
                        c.op('pe', lambda e: e.matmul(PL[p][:, :], lhsT=KTh[hb][:, kt * 128:(kt + 1) * 128], rhs=QTh[hb][:, :], start=True, stop=True),
                             [f'KTh{hb}', f'QTh{hb}'], [f'PL{p}'])
                    logits(0)
                    if nk > 1:
                        logits(1)
                    for kt in range(nk):
                        p = kt % 2
                        near = kt >= 4 * qs - 1
                        if near:
                            j = kt - (4 * qs - 1)
                            c.op('act', lambda e: e.activation(out=PTe[p][:], in_=PL[p][:, :], func=AF.Exp, scale=SCALE_B), [f'PL{p}'], [f'PTe{p}'])
                            c.op('pool', lambda e: e.tensor_tensor(out=PTm[p][:], in0=PTe[p][:], in1=ME[mset + j][:], op=ALU.mult),
                                 [f'PTe{p}', f'ME{mset + j}'], [f'PTm{p}'])
                        else:
                            c.op('act', lambda e: e.activation(out=PTe[p][:], in_=PL[p][:, :], func=AF.Exp, scale=SCALE_B, bias=cb[:, h:h + 1]),
                                 [f'PL{p}', 'cb'], [f'PTe{p}'])
                            c.op('pool', lambda e: e.tensor_tensor(out=PTm[p][:], in0=PTe[p][:], in1=mT_[:, kt, :], op=ALU.mult),
                                 [f'PTe{p}', mkey], [f'PTm{p}'])
                        if kt + 2 < nk:
                            logits(kt + 2)
                        for ti in range(4):
                            if kt > 4 * qs + ti:
                                continue
                            bk = ti // 2
                            c0 = (ti % 2) * 130
                            st = not started[bk]
                            started[bk] = True
                            c.op('pe', lambda e: e.matmul(PO[bk][:, c0:c0 + 129], lhsT=PTm[p][:, ti * 128:(ti + 1) * 128], rhs=Vh[hb][:, kt, 0:129],
                                                          start=st, stop=(kt == 4 * qs + ti), skip_group_check=True), [f'PTm{p}', f'Vh{hb}'], [f'PO{bk}'])
                        yield 1.25
                    for bk in range(2):
                        c.op('act', lambda e: e.copy(out=pos[bk][:], in_=PO[bk][:, 0:260]), [f'PO{bk}'], [f'pos{bk}'])
                        c.op('pool', lambda e: e.tensor_tensor(out=rden2[:, bk, :], in0=pos[bk][:, 128:259:130], in1=mone_t[:].to_broadcast([128, 2]),
                                                               op=ALU.pow), [f'pos{bk}', 'mone_t'], [f'rden2_{bk}'])
                        for t2 in range(2):
                            ti = bk * 2 + t2
                            c.op('pool', lambda e: e.tensor_scalar(out=obuf[:, ti, h * 128:(h + 1) * 128], in0=pos[bk][:, t2 * 130:t2 * 130 + 128],
                                                                   scalar1=rden2[:, bk, t2:t2 + 1], scalar2=None, op0=ALU.mult),
                                 [f'pos{bk}', f'rden2_{bk}'], ['obuf'])
                    yield 3.0
                c.dma('sp', ob_s[qs * 512:(qs + 1) * 512, :].rearrange("(ti p) c -> p ti c", p=128), obuf[:], ['obuf'], ['ob_s'], 'obuf')

            done_S, done_T, done_A = set(), set(), set()

            def stream_S():
                for g in range(32):
                    qs, qi = g // 4, g % 4
                    if g >= 2:
                        yield ('wait', lambda g=g: (g - 2) in done_T)
                    if qi == 0:
                        ib = qs % 2
                        c.dma('sp', iqb[ib][:], iqT_s.rearrange("(j r) t -> r j t", r=128)[:, :, qs * 512:(qs + 1) * 512],
                              ['iqT_s'], [f'iqb{ib}'], f'iqb{ib}')
                    yield from scoring(qs, qi)
                    done_S.add(g)

            def stream_T():
                for g in range(32):
                    qs, qi = g // 4, g % 4
                    yield ('wait', lambda g=g: g in done_S)
                    if qi == 0:
                        if qs >= 2:
                            yield ('wait', lambda qs=qs: (qs - 2) in done_A)
                        mT_ = maskT2[qs % 2]
                        for q3 in range(3):
                            c.op('dve', lambda e: e.memset(mT_[:, 4 * qs + q3 + 1:4 * qs + 4, q3 * 128:(q3 + 1) * 128], 0.0), [], [f'maskT{qs % 2}'])
                    yield from select(qs, qi)
                    done_T.add(g)

            def stream_A():
                for qs in range(8):
                    yield ('wait', lambda qs=qs: (4 * qs + 3) in done_T)
                    yield from gen_attn(qs)
                    done_A.add(qs)

            def run_streams(gens):
                st = [{'g': g, 't': 0.0, 'blk': None} for g in gens]
                while st:
                    ready = []
                    for x in st:
                        if x['blk'] is not None and x['blk']():
                            x['blk'] = None
                            others = [y['t'] for y in st if y is not x and y['blk'] is None]
                            if others:
                                x['t'] = max(x['t'], min(others))
                        if x['blk'] is None:
                            ready.append(x)
                    assert ready, "stream deadlock"
                    x = min(ready, key=lambda y: y['t'])
                    try:
                        r = next(x['g'])
                    except StopIteration:
                        st.remove(x)
                        continue
                    if isinstance(r, tuple):
                        if not r[1]():
                            x['blk'] = r[1]
                    else:
                        x['t'] += float(r or 1.0)
            run_streams([stream_S(), stream_T(), stream_A()])
            c.barrier()

        w_pre = ExitStack()
        W1 = sb(w_pre, "W1", [128, 8, 4096], BF16)
        with ExitStack() as p5:
            Wa = sb(p5, "Wa", [128, 8, 1024], BF16)
            Wb = sb(p5, "Wb", [128, 8, 1024], BF16)
            Wo = sb(p5, "Wo", [128, 8, 1024], BF16)
            n2w_bc = sb(p5, "n2w_bc", [128, D], F32)
            for wi, (wt, wd) in enumerate(((Wa, wpa_d), (Wb, wpb_d), (Wo, wo_d))):
                for hf in range(2):
                    c.dma('pool', wt[:, :, hf * 512:(hf + 1) * 512], wd.rearrange("(k p) c -> p k c", p=128)[:, :, hf * 512:(hf + 1) * 512],
                          [], [f'W5_{wi}_{hf}'], f'c{wi * 2 + hf}')
            c.dma('sp', n2w_bc[:], n2w_d.partition_broadcast(128), [], ['n2w_bc'], 'c6')
            for j in range(8):
                c.dma('pool', W1[:, :, j * 512:(j + 1) * 512], wf1_d.rearrange("(k p) f -> p k f", p=128)[:, :, j * 512:(j + 1) * 512],
                      [], [f'W1_{j}'], f'w1_{j % 4}')
            oin = [sb(p5, f"oin{i}", [128, 1024], BF16) for i in range(2)]
            oaT = sb(p5, "oaT", [128, 8, 512], BF16)
            obT = sb(p5, "obT", [128, 8, 512], BF16)
            g0 = sb(p5, "g0", [128, 8, 512], BF16)
            g1 = sb(p5, "g1", [128, 8, 512], BF16)
            mT = sb(p5, "mT", [128, 8, 512], BF16)
            t1f = [sb(p5, f"t1f{i}", [128, 512], F32) for i in range(2)]
            t2f = [sb(p5, f"t2f{i}", [128, 512], F32) for i in range(2)]
            xin = [sb(p5, f"xin{i}", [128, D], F32) for i in range(2)]
            x1t = [sb(p5, f"x1t{i}", [128, D], F32) for i in range(2)]
            h2b = [sb(p5, f"h2b{i}", [128, D], BF16) for i in range(2)]
            h2T = [sb(p5, f"h2T{i}", [128, 8, 128], BF16) for i in range(2)]
            junk5 = sb(p5, "junk5", [128, D], BF16)
            ss5 = sb(p5, "ss5", [128, NT], F32)
            rs5 = sb(p5, "rs5", [128, NT], F32)
            ptr = [ps(p5, f"ptr{i}", [128, 8, 128], BF16) for i in range(2)]
            ppas = [ps(p5, f"ppa{i}", [128, 512], F32) for i in range(2)]
            ppbs = [ps(p5, f"ppb{i}", [128, 512], F32) for i in range(2)]
            pout = [ps(p5, f"pout{i}", [128, 512], F32) for i in range(2)]
            nti = 0
            for blk in range(NB):
                csl = slice(blk * 512, (blk + 1) * 512)
                c.dma('sp', g0[:], gT_s[0:1024, csl].rearrange("(c p) t -> p c t", p=128), ['gT_s'], ['g0'], 'g0')
                c.dma('sp', g1[:], gT_s[1024:2048, csl].rearrange("(c p) t -> p c t", p=128), ['gT_s'], ['g1'], 'g1')
                for (src_s, dstT, key) in ((oa_s, oaT, 'oaT'), (ob_s, obT, 'obT')):
                    for ti in range(4):
                        tt = blk * 4 + ti
                        b = nti % 2
                        nti += 1
                        c.dma('sp', oin[b][:], src_s[tt * 128:(tt + 1) * 128, :], [src_s.tensor.name], [f'oin{b}'], f'oin{b}')
                        for k in range(8):
                            c.op('pe', lambda e: e.transpose(out=ptr[b][:, k, :], in_=oin[b][:, k * 128:(k + 1) * 128], identity=ident_b[:]),
                                 [f'oin{b}', 'ident_b'], [f'ptr{b}'])
                        evq = 'act' if nti % 2 == 0 else 'dve'
                        if evq == 'act':
                            c.op('act', lambda e: e.copy(out=dstT[:, :, ti * 128:(ti + 1) * 128], in_=ptr[b][:]), [f'ptr{b}'], [key])
                        else:
                            c.op('dve', lambda e: e.tensor_copy(out=dstT[:, :, ti * 128:(ti + 1) * 128], in_=ptr[b][:]), [f'ptr{b}'], [key])
                for dc in range(8):
                    tb = dc % 2
                    ppa, ppb = ppas[tb], ppbs[tb]
                    for k in range(8):
                        c.op('pe', lambda e: e.matmul(ppa[:, :], lhsT=Wa[:, k, dc * 128:(dc + 1) * 128], rhs=oaT[:, k, :], start=(k == 0), stop=(k == 7)),
                             [f'W5_0_{dc // 4}', 'oaT'], [f'ppa{tb}'])
                    for k in range(8):
                        c.op('pe', lambda e: e.matmul(ppb[:, :], lhsT=Wb[:, k, dc * 128:(dc + 1) * 128], rhs=obT[:, k, :], start=(k == 0), stop=(k == 7)),
                             [f'W5_1_{dc // 4}', 'obT'], [f'ppb{tb}'])
                    c.op('dve', lambda e: e.tensor_tensor(out=t1f[tb][:], in0=ppa[:, :], in1=g0[:, dc, :], op=ALU.mult), [f'ppa{tb}', 'g0'], [f't1f{tb}'])
                    c.op('dve', lambda e: e.tensor_tensor(out=t2f[tb][:], in0=ppb[:, :], in1=g1[:, dc, :], op=ALU.mult), [f'ppb{tb}', 'g1'], [f't2f{tb}'])
                    c.op('pool', lambda e: e.tensor_tensor(out=mT[:, dc, :], in0=t1f[tb][:], in1=t2f[tb][:], op=ALU.add),
                         [f't1f{tb}', f't2f{tb}'], ['mT'])
                for ti in range(4):
                    tt = blk * 4 + ti
                    xb = tt % 2
                    c.dma('sp', xin[xb][:], x_d[tt * 128:(tt + 1) * 128, :], [], [f'xin{xb}'], f'xin{xb}')
                    for hf in range(2):
                        for k in range(8):
                            c.op('pe', lambda e: e.matmul(pout[hf][:, :], lhsT=mT[:, k, ti * 128:(ti + 1) * 128], rhs=Wo[:, k, hf * 512:(hf + 1) * 512],
                                                          start=(k == 0), stop=(k == 7)), ['mT', f'W5_2_{hf}'], [f'pout{hf}'])
                        c.op('dve', lambda e: e.tensor_tensor(out=x1t[xb][:, hf * 512:(hf + 1) * 512], in0=pout[hf][:, :],
                                                              in1=xin[xb][:, hf * 512:(hf + 1) * 512], op=ALU.add),
                             [f'pout{hf}', f'xin{xb}'], [f'x1t{xb}'])
                    c.dma('sp', x1_s[tt * 128:(tt + 1) * 128, :], x1t[xb][:], [f'x1t{xb}'], ['x1_s'], f'x1t{xb}')
                    c.op('act', lambda e: e.activation(out=junk5[:], in_=x1t[xb][:], func=AF.Square, accum_out=ss5[:, tt:tt + 1]),
                         [f'x1t{xb}'], [f'ss5_{tt}', 'junk5'])
                    c.op('pool', lambda e: e.tensor_scalar(out=rs5[:, tt:tt + 1], in0=ss5[:, tt:tt + 1], scalar1=1.0 / D, scalar2=EPS,
                                                           op0=ALU.mult, op1=ALU.add), [f'ss5_{tt}'], [f'rs5_{tt}'])
                    c.op('pool', lambda e: e.tensor_tensor(out=rs5[:, tt:tt + 1], in0=rs5[:, tt:tt + 1], in1=mhalf_t[:], op=ALU.pow),
                         [f'rs5_{tt}', 'mhalf_t'], [f'rs5_{tt}'])
                    c.op('dve', lambda e: e.scalar_tensor_tensor(out=h2b[xb][:], in0=x1t[xb][:], scalar=rs5[:, tt:tt + 1], in1=n2w_bc[:],
                                                                 op0=ALU.mult, op1=ALU.mult), [f'x1t{xb}', f'rs5_{tt}', 'n2w_bc'], [f'h2b{xb}'])
                    for k in range(8):
                        c.op('pe', lambda e: e.transpose(out=ptr[xb][:, k, :], in_=h2b[xb][:, k * 128:(k + 1) * 128], identity=ident_b[:]),
                             [f'h2b{xb}', 'ident_b'], [f'ptr{xb}'])
                    c.op('act', lambda e: e.copy(out=h2T[xb][:], in_=ptr[xb][:]), [f'ptr{xb}'], [f'h2T{xb}'])
                    c.dma('sp', h2T_s[:, :, tt * 128:(tt + 1) * 128], h2T[xb][:], [f'h2T{xb}'], ['h2T_s'], f'h2T{xb}')
            c.barrier()

        with ExitStack() as p6:
            W2 = sb(p6, "W2", [128, 32, 1024], BF16)
            nfw_bc = sb(p6, "nfw_bc", [128, D], F32)
            for j in range(8):
                c.dma('pool', W2[:, j * 4:(j + 1) * 4, :], wf2_d.rearrange("(cc p) d -> p cc d", p=128)[:, j * 4:(j + 1) * 4, :],
                      [], [f'W2_{j}'], f'c{4 + j % 4}')
            c.dma('sp', nfw_bc[:], nfw_d.partition_broadcast(128), [], ['nfw_bc'], 'c8')
            TB = 256
            hin = [sb(p6, f"hin{i}", [128, 8, TB], BF16) for i in range(2)]
            aT = sb(p6, "aT", [128, 32, TB], BF16)
            rf = [sb(p6, f"rf{i}", [128, 2 * TB], F32) for i in range(2)]
            x1in = [sb(p6, f"x1in{i}", [128, D], F32) for i in range(2)]
            x2t = [sb(p6, f"x2t{i}", [128, D], F32) for i in range(2)]
            yt = x2t
            junk6 = sb(p6, "junk6", [128, D], BF16)
            ss6 = sb(p6, "ss6", [128, NT], F32)
            rs6 = sb(p6, "rs6", [128, NT], F32)
            pf1 = [ps(p6, f"pf1_{i}", [128, 512], F32) for i in range(3)]
            pf2 = [ps(p6, f"pf2_{i}", [128, 512], F32) for i in range(4)]
            for blk in range(T // TB):
                hb_ = blk % 2
                c.dma('sp', hin[hb_][:], h2T_s[:, :, blk * TB:(blk + 1) * TB], ['h2T_s'], [f'hin{hb_}'], f'hin{hb_}')
                for f2 in range(16):
                    pb = f2 % 3
                    for sub in range(2):
                        fc = f2 * 2 + sub
                        for k in range(8):
                            c.op('pe', lambda e: e.matmul(pf1[pb][:, sub * TB:(sub + 1) * TB], lhsT=W1[:, k, fc * 128:(fc + 1) * 128], rhs=hin[hb_][:, k, :],
                                                          start=(k == 0), stop=(k == 7)), [f'W1_{fc // 4}', f'hin{hb_}'], [f'pf1_{pb}'])
                    rb = f2 % 2
                    c.op('act', lambda e: e.activation(out=rf[rb][:], in_=pf1[pb][:, :], func=AF.Relu), [f'pf1_{pb}'], [f'rf{rb}'])
                    eng = 'dve' if f2 % 2 == 0 else 'pool'
                    c.op(eng, lambda e: e.tensor_tensor(out=aT[:, f2 * 2:f2 * 2 + 2, :].rearrange("p a t -> p (a t)"), in0=rf[rb][:], in1=rf[rb][:], op=ALU.mult),
                         [f'rf{rb}'], ['aT'])
                for ti in range(TB // 128):
                    tt = blk * (TB // 128) + ti
                    xb = tt % 2
                    c.dma('sp', x1in[xb][:], x1_s[tt * 128:(tt + 1) * 128, :], ['x1_s'], [f'x1in{xb}'], f'x1in{xb}')
                    for hf in range(2):
                        pb = (ti * 2 + hf) % 4
                        for fc in range(32):
                            c.op('pe', lambda e: e.matmul(pf2[pb][:, :], lhsT=aT[:, fc, ti * 128:(ti + 1) * 128], rhs=W2[:, fc, hf * 512:(hf + 1) * 512],
                                                          start=(fc == 0), stop=(fc == 31)), ['aT', f'W2_{fc // 4}'], [f'pf2_{pb}'])
                        c.op('dve', lambda e: e.tensor_tensor(out=x2t[xb][:, hf * 512:(hf + 1) * 512], in0=pf2[pb][:, :],
                                                              in1=x1in[xb][:, hf * 512:(hf + 1) * 512], op=ALU.add),
                             [f'pf2_{pb}', f'x1in{xb}'], [f'x2t{xb}'])
                    c.op('act', lambda e: e.activation(out=junk6[:], in_=x2t[xb][:], func=AF.Square, accum_out=ss6[:, tt:tt + 1]),
                         [f'x2t{xb}'], [f'ss6_{tt}', 'junk6'])
                    c.op('pool', lambda e: e.tensor_scalar(out=rs6[:, tt:tt + 1], in0=ss6[:, tt:tt + 1], scalar1=1.0 / D, scalar2=EPS,
                                                           op0=ALU.mult, op1=ALU.add), [f'ss6_{tt}'], [f'rs6_{tt}'])
                    c.op('pool', lambda e: e.tensor_tensor(out=rs6[:, tt:tt + 1], in0=rs6[:, tt:tt + 1], in1=mhalf_t[:], op=ALU.pow),
                         [f'rs6_{tt}', 'mhalf_t'], [f'rs6_{tt}'])
                    c.op('dve', lambda e: e.scalar_tensor_tensor(out=yt[xb][:], in0=x2t[xb][:], scalar=rs6[:, tt:tt + 1], in1=nfw_bc[:],
                                                                 op0=ALU.mult, op1=ALU.mult), [f'x2t{xb}', f'rs6_{tt}', 'nfw_bc'], [f'x2t{xb}'])
                    c.dma('sp', y_d[tt * 128:(tt + 1) * 128, :], yt[xb][:], [f'x2t{xb}'], ['y_d'], f'yt{xb}')
            c.barrier()
        w_pre.close()
        c.barrier(engines=('sp',))
    print(f"[build] instructions={c.nins} waits={c.nwait}")
    return nc


def make_in_maps(inputs):
    f = lambda a: np.ascontiguousarray(np.asarray(a, dtype=np.float32))
    cst = _consts()
    shared = {
        "norm1_w": f(inputs["norm1_w"]).reshape(1, D),
        "w_in": f(inputs["w_in"])[0],
        "conv_wt": np.ascontiguousarray(f(inputs["conv_a_w"])[0].T),
        "a_log": f(inputs["a_log"]).reshape(8, 1),
        "dt_bias": f(inputs["dt_bias"]).reshape(8, 1),
        "norm_a_w": f(inputs["norm_a_w"]).reshape(1, 128),
        "rel_bias_table": f(inputs["rel_bias_table"]),
        "w_gate": f(inputs["w_gate"])[0],
        "b_gate_t": np.ascontiguousarray(f(inputs["b_gate"]).reshape(16, 128).T),
        "w_proj_a": f(inputs["w_proj_a"])[0],
        "w_proj_b": f(inputs["w_proj_b"])[0],
        "w_out": f(inputs["w_out"])[0],
        "norm2_w": f(inputs["norm2_w"]).reshape(1, D),
        "w_ff1": f(inputs["w_ff1"])[0],
        "w_ff2": f(inputs["w_ff2"])[0],
        "norm_final_w": f(inputs["norm_final_w"]).reshape(1, D),
        "c_ident": cst['ident'],
        "c_sel": cst['sel'],
        "c_negm": cst['negm'],
        "c_oh": cst['oh'],
        "c_J": cst['J'],
        "c_visneg": cst['visneg'],
        "c_pw": cst['pw'],
    }
    x = f(inputs["x"])
    return [dict(shared, x=x[b]) for b in range(x.shape[0])]


def kernel(**inputs):
    nc = build_program(debug=False)
    in_maps = make_in_maps(inputs)
    res = run_bass_kernel_spmd(nc, in_maps, core_ids=list(range(8)))
    return np.stack([np.asarray(r["y"], dtype=np.float32) for r in res.results], axis=0)
```
